# Optimizing a Trainium2 kernel written in Bass

```python
import math
import jax, jax.numpy as jnp
from jax import lax
import numpy as np

D_MODEL = 1024
BATCH = 16
SEQ = 4096
DEPTH = 1

N_MEM = 256
D_MIX = D_MODEL
D_MLSTM = D_MIX // 2
N_MLSTM_HEADS = 4
MLSTM_HEAD_DIM = D_MLSTM // N_MLSTM_HEADS
MLSTM_CHUNK = 128
CONV_WIDTH = 5
D_GMLP = D_MIX - D_MLSTM
N_GMLP_GROUPS = 4
GMLP_GROUP_DIM = D_GMLP // N_GMLP_GROUPS
GMLP_CHUNK = 128
N_GATES = 4 * N_MLSTM_HEADS
PROJ_COLS = 4 * D_MLSTM + N_GATES + 2 * D_GMLP
N_XATTN_HEADS = 4
XATTN_HEAD_DIM = D_MODEL // N_XATTN_HEADS
N_EXPERT_GROUPS = 4
EXPERTS_PER_GROUP = 8
N_EXPERTS = N_EXPERT_GROUPS * EXPERTS_PER_GROUP
TOP_K_IN_GROUP = 2
D_EXPERT = D_MODEL // 2
MOE_BLOCK = 128
RMS_EPS = 1e-6
LN_EPS = 1e-5
NEG_INIT = -1e30

kernel_name = 'hybrid_mlstm_gmlp_xattn_hmoe_encoder'


def rms_norm(x, g):
    xf = x.astype(jnp.float32)
    y = xf * lax.rsqrt(jnp.mean(xf * xf, axis=-1, keepdims=True) + RMS_EPS)
    return (y * g.astype(jnp.float32)).astype(x.dtype)


def layer_norm(x, g, b):
    xf = x.astype(jnp.float32)
    mu = jnp.mean(xf, axis=-1, keepdims=True)
    xc = xf - mu
    y = xc * lax.rsqrt(jnp.mean(xc * xc, axis=-1, keepdims=True) + LN_EPS)
    return (y * g.astype(jnp.float32) + b.astype(jnp.float32)).astype(x.dtype)


def mlstm_chunkwise(q, k, v, i_pre, f_pre):
    Bq, H, S, dh = q.shape
    L = MLSTM_CHUNK
    NC = S // L
    q = q.reshape(Bq, H, NC, L, dh)
    k = k.reshape(Bq, H, NC, L, dh) * (dh ** -0.5)
    v = v.reshape(Bq, H, NC, L, dh)
    ig = i_pre.reshape(Bq, H, NC, L)
    b = jnp.cumsum(jax.nn.log_sigmoid(f_pre.reshape(Bq, H, NC, L)), axis=-1)
    F = b[..., -1]
    a = F[..., None] - b + ig
    m_loc = jnp.max(a, axis=-1)
    kw = k * jnp.exp(a - m_loc[..., None])[..., None]
    C_loc = jnp.einsum('bhcsd,bhcse->bhcde', kw, v)
    n_loc = jnp.sum(kw, axis=-2)

    def step(carry, inp):
        C, n, m = carry
        F_c, m_c, C_c, n_c = inp
        m_new = jnp.maximum(F_c + m, m_c)
        s_old = jnp.exp(F_c + m - m_new)
        s_new = jnp.exp(m_c - m_new)
        C_new = s_old[..., None, None] * C + s_new[..., None, None] * C_c
        n_new = s_old[..., None] * n + s_new[..., None] * n_c
        return (C_new, n_new, m_new), (C, n, m)

    init = (jnp.zeros((Bq, H, dh, dh), jnp.float32),
            jnp.zeros((Bq, H, dh), jnp.float32),
            jnp.full((Bq, H), NEG_INIT, jnp.float32))
    xs = (jnp.moveaxis(F, 2, 0), jnp.moveaxis(m_loc, 2, 0),
          jnp.moveaxis(C_loc, 2, 0), jnp.moveaxis(n_loc, 2, 0))
    _, (C_prev, n_prev, m_prev) = lax.scan(step, init, xs)
    C_prev = jnp.moveaxis(C_prev, 0, 2)
    n_prev = jnp.moveaxis(n_prev, 0, 2)
    m_prev = jnp.moveaxis(m_prev, 0, 2)

    mask = jnp.tril(jnp.ones((L, L), dtype=bool))
    Dm = jnp.where(mask, b[..., :, None] - b[..., None, :] + ig[..., None, :], -jnp.inf)
    inter = b + m_prev[..., None]
    m_t = jnp.maximum(inter, jnp.max(Dm, axis=-1))
    Wqk = jnp.exp(Dm - m_t[..., None]) * jnp.einsum('bhctd,bhcsd->bhcts', q, k)
    s_inter = jnp.exp(inter - m_t)
    num = (jnp.einsum('bhcts,bhcse->bhcte', Wqk, v)
           + s_inter[..., None] * jnp.einsum('bhctd,bhcde->bhcte', q, C_prev))
    den = jnp.sum(Wqk, axis=-1) + s_inter * jnp.einsum('bhctd,bhcd->bhct', q, n_prev)
    h = num / jnp.maximum(jnp.abs(den), jnp.exp(-m_t))[..., None]
    return h.reshape(Bq, H, S, dh)


def parallel_mixer(h, w_in, conv_w, conv_b, gate_b, g_head, ln_v_g, ln_v_b, w_s, b_s, w_out):
    Bq, S, _ = h.shape
    z = h @ w_in
    cuts = [D_MLSTM, 2 * D_MLSTM, 3 * D_MLSTM, 4 * D_MLSTM,
            4 * D_MLSTM + N_GATES, 4 * D_MLSTM + N_GATES + D_GMLP]
    q, k, v, o, gates, gu, gv = jnp.split(z, cuts, axis=-1)

    qk = lax.conv_general_dilated(jnp.concatenate([q, k], axis=-1), conv_w, (1,), 'SAME',
                                  dimension_numbers=('NWC', 'WIO', 'NWC'),
                                  feature_group_count=2 * D_MLSTM)
    qk = jax.nn.silu(qk + conv_b)
    q, k = qk[..., :D_MLSTM], qk[..., D_MLSTM:]

    def heads(t):
        return t.reshape(Bq, S, N_MLSTM_HEADS, MLSTM_HEAD_DIM).transpose(0, 2, 1, 3).astype(jnp.float32)

    qh, kh, vh = heads(q), heads(k), heads(v)
    g = (gates.astype(jnp.float32) + gate_b.astype(jnp.float32)).reshape(Bq, S, 4, N_MLSTM_HEADS)
    g = g.transpose(2, 0, 3, 1)
    i_f, f_f, i_b, f_b = g[0], g[1], g[2], g[3]

    def flip(t):
        return jnp.flip(t, axis=2)

    h_fwd = mlstm_chunkwise(qh, kh, vh, i_f, f_f)
    h_bwd = flip(mlstm_chunkwise(flip(qh), flip(kh), flip(vh), flip(i_b), flip(f_b)))
    hs = h_fwd + h_bwd
    gh = g_head.astype(jnp.float32).reshape(N_MLSTM_HEADS, 1, MLSTM_HEAD_DIM)
    hs = hs * lax.rsqrt(jnp.mean(hs * hs, axis=-1, keepdims=True) + RMS_EPS) * gh
    hs = hs.transpose(0, 2, 1, 3).reshape(Bq, S, D_MLSTM)
    y_mlstm = (jax.nn.sigmoid(o.astype(jnp.float32)) * hs).astype(h.dtype)

    gu = jax.nn.gelu(gu)
    gv = layer_norm(jax.nn.gelu(gv), ln_v_g, ln_v_b)
    NC = S // GMLP_CHUNK
    gv = gv.reshape(Bq, NC, GMLP_CHUNK, N_GMLP_GROUPS, GMLP_GROUP_DIM)
    sp = jnp.einsum('gts,bcsgd->bctgd', w_s, gv) + b_s.T[:, :, None]
    y_gmlp = gu * sp.reshape(Bq, S, D_GMLP)

    return jnp.concatenate([y_mlstm, y_gmlp], axis=-1) @ w_out


def memory_cross_attention(h, hm, w_q, w_kv, w_o):
    Bq, S, _ = h.shape
    M = hm.shape[1]
    q = (h @ w_q).reshape(Bq, S, N_XATTN_HEADS, XATTN_HEAD_DIM)
    kv = (hm @ w_kv).reshape(Bq, M, 2, N_XATTN_HEADS, XATTN_HEAD_DIM)
    k, v = kv[:, :, 0], kv[:, :, 1]
    s = jnp.einsum('bshd,bmhd->bhsm', q, k).astype(jnp.float32) * (XATTN_HEAD_DIM ** -0.5)
    p = jax.nn.softmax(s, axis=-1).astype(h.dtype)
    o = jnp.einsum('bhsm,bmhd->bshd', p, v).reshape(Bq, S, N_XATTN_HEADS * XATTN_HEAD_DIM)
    return o @ w_o


def hier_moe(h, w_rg, b_rg, w_re, b_re, w_gate, w_up, w_down):
    T, D = h.shape
    g_logits = (h @ w_rg).astype(jnp.float32) + b_rg.astype(jnp.float32)
    p_group = jax.nn.softmax(g_logits, axis=-1)
    p_top, g_idx = lax.top_k(p_group, 1)
    e_logits = ((h @ w_re).astype(jnp.float32) + b_re.astype(jnp.float32)).reshape(
        T, N_EXPERT_GROUPS, EXPERTS_PER_GROUP)
    e_logits = jnp.take_along_axis(e_logits, g_idx[:, :, None], axis=1)[:, 0]
    e_top, e_local = lax.top_k(e_logits, TOP_K_IN_GROUP)
    weights = p_top * jax.nn.softmax(e_top, axis=-1)
    expert_id = g_idx * EXPERTS_PER_GROUP + e_local

    A = T * TOP_K_IN_GROUP
    e_flat = expert_id.reshape(A)
    t_flat = jnp.repeat(jnp.arange(T), TOP_K_IN_GROUP)
    w_flat = weights.reshape(A)
    order = jnp.argsort(e_flat)
    e_sorted, t_sorted, w_sorted = e_flat[order], t_flat[order], w_flat[order]
    counts = jnp.bincount(e_flat, length=N_EXPERTS)
    padded = ((counts + MOE_BLOCK - 1) // MOE_BLOCK) * MOE_BLOCK
    start = jnp.cumsum(counts) - counts
    pend = jnp.cumsum(padded)
    pstart = pend - padded
    dest = pstart[e_sorted] + (jnp.arange(A) - start[e_sorted])
    NB = A // MOE_BLOCK + N_EXPERTS
    P = NB * MOE_BLOCK
    x_disp = jnp.zeros((P, D), h.dtype).at[dest].set(h[t_sorted])
    blk_e = jnp.clip(jnp.searchsorted(pend, jnp.arange(NB) * MOE_BLOCK, side='right'), 0, N_EXPERTS - 1)

    def expert_block(args):
        xb, e = args
        return (jax.nn.silu(xb @ w_gate[e]) * (xb @ w_up[e])) @ w_down[e]

    y_disp = lax.map(expert_block, (x_disp.reshape(NB, MOE_BLOCK, D), blk_e)).reshape(P, D)
    contrib = (w_sorted[:, None] * y_disp[dest].astype(jnp.float32))
    out = jnp.zeros((T, D), jnp.float32).at[t_sorted].add(contrib)
    return out.astype(h.dtype)


def setup_inputs(seed: int = 0) -> dict:
    key = jax.random.key(seed)
    ks = jax.random.split(key, 32)

    def nrm(k, shape, scale):
        return jax.random.normal(k, shape, jnp.float32) * scale

    def gain(k, shape):
        return 1.0 + 0.1 * jax.random.normal(k, shape, jnp.float32)

    H = N_MLSTM_HEADS
    f_bias = jnp.linspace(3.0, 6.0, H, dtype=jnp.float32)
    gk = jax.random.split(ks[6], 4)
    gate_b = jnp.concatenate([
        nrm(gk[0], (DEPTH, H), 0.1),
        f_bias + nrm(gk[1], (DEPTH, H), 0.1),
        nrm(gk[2], (DEPTH, H), 0.1),
        f_bias + nrm(gk[3], (DEPTH, H), 0.1)], axis=-1)
    return {
        'x': nrm(ks[0], (BATCH, SEQ, D_MODEL), 1.0),
        'mem': nrm(ks[1], (BATCH, N_MEM, D_MODEL), 1.0),
        'g_mix': gain(ks[2], (DEPTH, D_MODEL)),
        'w_in': nrm(ks[3], (DEPTH, D_MODEL, PROJ_COLS), D_MODEL ** -0.5),
        'conv_w': nrm(ks[4], (DEPTH, CONV_WIDTH, 1, 2 * D_MLSTM), CONV_WIDTH ** -0.5),
        'conv_b': nrm(ks[5], (DEPTH, 2 * D_MLSTM), 0.01),
        'gate_b': gate_b,
        'g_head': gain(ks[7], (DEPTH, D_MLSTM)),
        'ln_v_g': gain(ks[8], (DEPTH, D_GMLP)),
        'ln_v_b': nrm(ks[9], (DEPTH, D_GMLP), 0.01),
        'w_s': nrm(ks[10], (DEPTH, N_GMLP_GROUPS, GMLP_CHUNK, GMLP_CHUNK), GMLP_CHUNK ** -0.5),
        'b_s': gain(ks[11], (DEPTH, N_GMLP_GROUPS, GMLP_CHUNK)),
        'w_out': nrm(ks[12], (DEPTH, D_MIX, D_MODEL), D_MIX ** -0.5),
        'g_xattn': gain(ks[13], (DEPTH, D_MODEL)),
        'g_mem': gain(ks[14], (DEPTH, D_MODEL)),
        'w_q_x': nrm(ks[15], (DEPTH, D_MODEL, N_XATTN_HEADS * XATTN_HEAD_DIM), D_MODEL ** -0.5),
        'w_kv_x': nrm(ks[16], (DEPTH, D_MODEL, 2 * N_XATTN_HEADS * XATTN_HEAD_DIM), D_MODEL ** -0.5),
        'w_o_x': nrm(ks[17], (DEPTH, N_XATTN_HEADS * XATTN_HEAD_DIM, D_MODEL), D_MODEL ** -0.5),
        'g_moe': gain(ks[18], (DEPTH, D_MODEL)),
        'w_rg': nrm(ks[19], (DEPTH, D_MODEL, N_EXPERT_GROUPS), D_MODEL ** -0.5),
        'b_rg': nrm(ks[20], (DEPTH, N_EXPERT_GROUPS), 0.01),
        'w_re': nrm(ks[21], (DEPTH, D_MODEL, N_EXPERTS), D_MODEL ** -0.5),
        'b_re': nrm(ks[22], (DEPTH, N_EXPERTS), 0.01),
        'w_gate': nrm(ks[23], (DEPTH, N_EXPERTS, D_MODEL, D_EXPERT), D_MODEL ** -0.5),
        'w_up': nrm(ks[24], (DEPTH, N_EXPERTS, D_MODEL, D_EXPERT), D_MODEL ** -0.5),
        'w_down': nrm(ks[25], (DEPTH, N_EXPERTS, D_EXPERT, D_MODEL), D_EXPERT ** -0.5),
        'g_final': gain(ks[26], (D_MODEL,)),
    }


def reference(x, mem, g_mix, w_in, conv_w, conv_b, gate_b, g_head, ln_v_g, ln_v_b, w_s, b_s, w_out,
              g_xattn, g_mem, w_q_x, w_kv_x, w_o_x, g_moe, w_rg, b_rg, w_re, b_re,
              w_gate, w_up, w_down, g_final):
    Bq, S, D = x.shape
    for l in range(DEPTH):
        x = x + parallel_mixer(rms_norm(x, g_mix[l]), w_in[l], conv_w[l], conv_b[l], gate_b[l],
                               g_head[l], ln_v_g[l], ln_v_b[l], w_s[l], b_s[l], w_out[l])
        x = x + memory_cross_attention(rms_norm(x, g_xattn[l]), rms_norm(mem, g_mem[l]),
                                       w_q_x[l], w_kv_x[l], w_o_x[l])
        x = x + hier_moe(rms_norm(x, g_moe[l]).reshape(Bq * S, D), w_rg[l], b_rg[l], w_re[l], b_re[l],
                         w_gate[l], w_up[l], w_down[l]).reshape(Bq, S, D)
    return rms_norm(x, g_final)
```

```python
import numpy as np
from contextlib import ExitStack
import concourse.bass as bass
import concourse.mybir as mybir
from concourse.bass_utils import run_bass_kernel_spmd

F32 = mybir.dt.float32
BF16 = mybir.dt.bfloat16
I32 = mybir.dt.int32
U32 = mybir.dt.uint32
ALU = mybir.AluOpType
AF = mybir.ActivationFunctionType
AX = mybir.AxisListType

ENGS = ("pe", "act", "dve", "pool", "sp")
EPOCH = 8000
DMA_EPOCH = 30000

NCORES = 8
D = 1024
NTOK = 8192
NT = NTOK // 128
SEQ = 4096
NCH = 32
PROJ = 3088
CAP = 1280
NSLOT = 32 * CAP
SBUF_BASE = 16512
SBUF_END = 229376
SERIAL = False
DYN_SKIP = True


class _Op:
    __slots__ = ("fn", "deps", "signal", "dma_key", "dma_val", "sigval", "sec")

    def __init__(self, fn, deps, dma_key=None, dma_val=None):
        self.fn = fn
        self.deps = deps
        self.signal = False
        self.dma_key = dma_key
        self.dma_val = dma_val
        self.sigval = None
        self.sec = None


class Sched:
    def __init__(self, nc):
        self.nc = nc
        self.ops = {e: [] for e in ENGS}
        self.last_w = {}
        self.readers = {}
        self.dma_cnt = {}
        self.secs = []
        self.cur_sec = None

    def section_begin(self, cnt_ap, thr, tag=None):
        self.secs.append((cnt_ap, thr, tag))
        self.cur_sec = len(self.secs) - 1

    def section_end(self):
        self.cur_sec = None

    def _deps(self, eng, r, w, is_dma):
        deps = []
        for k in r:
            t = self.last_w.get(k)
            if t is not None:
                deps.append((t, "raw"))
        for k in w:
            t = self.last_w.get(k)
            if t is not None:
                deps.append((t, "waw"))
            for t in self.readers.get(k, ()):
                deps.append((t, "war"))
        out = []
        seen = set()
        for t, kind in deps:
            if t in seen:
                continue
            if t[0] == "c" and t[1] == eng and not is_dma:
                if eng == "pe" or kind == "war":
                    continue
            seen.add(t)
            out.append(t)
        best = {}
        res = []
        for t in out:
            if t[0] == "c":
                if t[1] not in best or best[t[1]][2] < t[2]:
                    best[t[1]] = t
            else:
                res.append(t)
        return res + list(best.values())

    def _commit(self, tok, r, w):
        for k in w:
            self.last_w[k] = tok
            self.readers[k] = []
        for k in r:
            self.readers.setdefault(k, []).append(tok)

    serial = False

    def _all_toks(self, eng):
        toks = []
        for e in ENGS:
            if e == eng and e == "pe":
                continue
            n = len(self.ops[e])
            for i in range(n - 1, -1, -1):
                if self.ops[e][i].dma_key is None and self.ops[e][i].fn is not None:
                    toks.append(("c", e, i))
                    break
        for key, (gen, cnt) in self.dma_cnt.items():
            toks.append(("d", key, gen, cnt))
        return toks

    def op(self, eng, fn, r=(), w=()):
        deps = self._deps(eng, r, w, False)
        if self.serial is True or self.serial == eng:
            deps = self._all_toks(eng)
        idx = len(self.ops[eng])
        o = _Op(fn, deps)
        o.sec = self.cur_sec
        self.ops[eng].append(o)
        self._commit(("c", eng, idx), r, w)

    def dma(self, eng, fn, r=(), w=(), key=None):
        assert key is not None
        deps = self._deps(eng, r, w, True)
        if self.serial is True or self.serial == "dma":
            deps = self._all_toks(None)
        gen, cnt = self.dma_cnt.get(key, (0, 0))
        if cnt + 16 > DMA_EPOCH:
            gen, cnt = gen + 1, 0
        cnt += 16
        self.dma_cnt[key] = (gen, cnt)
        o = _Op(fn, deps, dma_key=(key, gen), dma_val=cnt)
        o.sec = self.cur_sec
        self.ops[eng].append(o)
        self._commit(("d", key, gen, cnt), r, w)

    def barrier(self):
        toks = []
        for e in ENGS:
            n = len(self.ops[e])
            for i in range(n - 1, -1, -1):
                if self.ops[e][i].dma_key is None and self.ops[e][i].fn is not None:
                    toks.append(("c", e, i))
                    break
        for key, (gen, cnt) in self.dma_cnt.items():
            toks.append(("d", key, gen, cnt))
        for e in ENGS:
            deps = [t for t in toks if not (t[0] == "c" and t[1] == e)]
            self.ops[e].append(_Op(None, deps))
        self.last_w = {}
        self.readers = {}

    def emit(self, stack):
        nc = self.nc
        for e in ENGS:
            for o in self.ops[e]:
                for t in o.deps:
                    if t[0] == "c":
                        self.ops[t[1]][t[2]].signal = True
        csem = {}
        for e in ENGS:
            cnt = 0
            for o in self.ops[e]:
                if o.signal:
                    cnt += 1
                    o.sigval = cnt
            nep = max((cnt + EPOCH - 1) // EPOCH, 1)
            csem[e] = [stack.enter_context(nc.semaphore(f"c_{e}_{i}")) for i in range(nep)]
        dsem = {}
        for e in ENGS:
            for o in self.ops[e]:
                if o.dma_key is not None and o.dma_key not in dsem:
                    dsem[o.dma_key] = stack.enter_context(nc.semaphore(f"d{len(dsem)}"))
        self.n_sems = sum(len(v) for v in csem.values()) + len(dsem)

        def sem_of(e, sigval):
            ep = (sigval - 1) // EPOCH
            return csem[e][ep], sigval - ep * EPOCH

        block = stack.enter_context(nc.Block())
        ops = self.ops

        secs = self.secs

        def run(e, engobj):
            waited_c = {x: 0 for x in ENGS}
            waited_d = {}
            creg = [None]

            def emit_one(o):
                for t in o.deps:
                    if t[0] == "c":
                        sv = ops[t[1]][t[2]].sigval
                        if sv <= waited_c[t[1]]:
                            continue
                        waited_c[t[1]] = sv
                        s_, v_ = sem_of(t[1], sv)
                        engobj.wait_ge(s_, v_)
                    else:
                        k = (t[1], t[2])
                        if waited_d.get(k, 0) >= t[3]:
                            continue
                        waited_d[k] = t[3]
                        engobj.wait_ge(dsem[k], t[3])
                if o.fn is None:
                    return
                ins = o.fn(engobj)
                if o.dma_key is not None:
                    ins.then_inc(dsem[o.dma_key], 16)
                elif o.signal:
                    s_, v_ = sem_of(e, o.sigval)
                    ins.then_inc(s_, 1)

            lst = ops[e]
            i = 0
            while i < len(lst):
                o = lst[i]
                if o.sec is None:
                    emit_one(o)
                    i += 1
                    continue
                j = i
                while j < len(lst) and lst[j].sec == o.sec:
                    j += 1
                group = lst[i:j]
                i = j
                cnt_ap, thr, tag = secs[o.sec]
                if creg[0] is None:
                    creg[0] = engobj.alloc_register("secreg")
                    creg.append(object())
                if tag is None or creg[1] != tag:
                    engobj.reg_load(creg[0], cnt_ap)
                    creg[1] = tag
                ccomp = {}
                dcomp = {}
                for g in group:
                    if g.dma_key is not None:
                        ent = dcomp.setdefault(g.dma_key, [dsem[g.dma_key], 0, g.dma_val - 16])
                        ent[1] += 16
                    elif g.signal:
                        s_, v_ = sem_of(e, g.sigval)
                        ent = ccomp.setdefault(id(s_), [s_, 0, v_ - 1])
                        ent[1] += 1
                saved_c = dict(waited_c)
                saved_d = dict(waited_d)
                with engobj.If_lt(creg[0], thr + 1):
                    for (s_, n_, pre_) in list(ccomp.values()) + list(dcomp.values()):
                        if pre_ > 0:
                            engobj.wait_ge(s_, pre_)
                        engobj.sem_inc(s_, n_)
                with engobj.Else():
                    for g in group:
                        emit_one(g)
                waited_c.clear()
                waited_c.update(saved_c)
                waited_d.clear()
                waited_d.update(saved_d)

        @block.tensor
        def _(eng):
            run("pe", eng)

        @block.scalar
        def _(eng):
            run("act", eng)

        @block.vector
        def _(eng):
            run("dve", eng)

        @block.gpsimd
        def _(eng):
            run("pool", eng)

        @block.sync
        def _(eng):
            run("sp", eng)


_BC = {}


def _bcreg(e):
    k = id(e)
    if k not in _BC:
        r = e.alloc_register("bcreg")
        e.reg_mov(r, NSLOT - 1)
        _BC[k] = (e, r)
    return _BC[k][1]


class Alloc:
    def __init__(self, nc):
        self.nc = nc
        self.off = SBUF_BASE
        self.n = 0

    def mark(self):
        return self.off

    def reset(self, m):
        self.off = m

    def __call__(self, name, shape, dt):
        esz = 2 if dt == BF16 else 4
        nb = int(np.prod(shape[1:])) * esz
        nb = (nb + 63) // 64 * 64
        assert self.off + nb <= SBUF_END, (name, self.off, nb)
        self.n += 1
        t = self.nc.alloc_sbuf_tensor_at(f"{name}_{self.n}", list(shape), dt, offset=self.off)
        self.off += nb
        return t


VOFF = {}
_o = 0
for _n, _l in [("g_mix", 1024), ("g_xattn", 1024), ("g_mem", 1024), ("g_moe", 1024), ("g_final", 1024),
               ("ln_g", 512), ("ln_b", 512), ("g_head", 512), ("gate_b", 16), ("b_r", 36), ("iota", 32)]:
    VOFF[_n] = (_o, _l)
    _o += _l
NVEC = _o
NCOL = 8 * 5 + 8 + 4


def build(stage=3, debug=False):
    nc = bass.Bass("TRN2", target_bir_lowering=False)

    def din(name, shape, dt=F32):
        return nc.dram_tensor(name, list(shape), dt, kind="ExternalInput").ap()

    x_d = din("x", [NTOK, D])
    mem_d = din("mem", [512, D])
    w_in_d = din("w_in", [D, PROJ])
    w_out_d = din("w_out", [D, D])
    w_q_d = din("w_q", [D, D])
    w_kv_d = din("w_kv", [D, 2 * D])
    w_o_d = din("w_o", [D, D])
    w_r_d = din("w_r", [D, 36])
    w_gate_d = din("w_gate", [32, D, 512])
    w_up_d = din("w_up", [32, D, 512])
    w_down_d = din("w_down", [32, 512, D])
    w_sT_d = din("w_sT", [128, 4, 128])
    vec_d = din("vec", [128, NVEC])
    col_d = din("col", [128, NCOL])
    cst_d = din("cst", [128, 5 * 128])
    out_d = nc.dram_tensor("out", [NTOK, D], F32, kind="ExternalOutput").ap()

    SK = "ExternalOutput" if debug else "Internal"
    zqk_d = nc.dram_tensor("zqk_s", [D, NTOK], BF16, kind=SK).ap()
    v_d = nc.dram_tensor("v_s", [NTOK, 512], BF16, kind=SK).ap()
    og_d = nc.dram_tensor("og_s", [NTOK, 512], F32, kind=SK).ap()
    y_d = nc.dram_tensor("y_s", [NTOK, D], BF16, kind=SK).ap()
    xd_d = nc.dram_tensor("xd_s", [NSLOT, D], BF16, kind="Internal").ap()
    yd_d = nc.dram_tensor("yd_s", [NSLOT, D], F32, kind="Internal").ap()

    S = Sched(nc)
    S.serial = SERIAL
    A = Alloc(nc)
    ps = [nc.alloc_psum_tensor(f"ps{i}", [128, 512], F32) for i in range(8)]
    PK = [f"ps{i}" for i in range(8)]

    cst = A("cst", [128, 640], F32)
    ident_b = A("identb", [128, 128], BF16)
    ones_b = A("onesb", [128, 128], BF16)
    strict_b = A("strictb", [128, 128], BF16)
    col = A("col", [128, NCOL], F32)
    gates_all = A("gates", [128, NT, 16], F32)
    slots_i = A("slots", [128, NT, 2], I32)
    wts = A("wts", [128, NT, 2], F32)
    cnt_i = A("cnt_i", [128, 32], I32)
    ident_f = cst[:, 0:128]
    maskf = cst[:, 128:256]
    maskb = cst[:, 256:384]
    ones_f = cst[:, 384:512]
    strict = cst[:, 512:640]

    S.dma("sp", lambda e: e.dma_start(out=cst[:], in_=cst_d), w=["cst"], key="cst")
    S.dma("sp", lambda e: e.dma_start(out=col[:], in_=col_d), w=["col"], key="col")
    S.op("dve", lambda e: e.tensor_copy(out=ident_b[:], in_=cst[:, 0:128]), r=["cst"], w=["identb"])
    S.op("dve", lambda e: e.tensor_copy(out=ones_b[:], in_=cst[:, 384:512]), r=["cst"], w=["onesb"])
    S.op("dve", lambda e: e.tensor_copy(out=strict_b[:], in_=cst[:, 512:640]), r=["cst"], w=["strictb"])

    PM = A.mark()

    def load_w(dst, src_d, key, nk, eng="pool"):
        for kc in range(nk):
            S.dma(eng, lambda e, kc=kc: e.dma_start(out=dst[:, kc, :], in_=src_d[kc * 128:(kc + 1) * 128, :]),
                  w=[key], key=key)

    _cast_rr = [0]

    def load_w_fast(dst, src_d, key, nk, stgs, queue="sp"):
        n_ = dst.shape[2]
        for kc in range(nk):
            si = _cast_rr[0] % len(stgs)
            stg_ap, skey = stgs[si]
            eng = ("dve", "act", "pool")[_cast_rr[0] % 3]
            _cast_rr[0] += 1
            S.dma(queue, lambda e, kc=kc, stg_ap=stg_ap: e.dma_start(out=stg_ap[:, 0:n_], in_=src_d[kc * 128:(kc + 1) * 128, :]),
                  w=[skey], key=skey)
            if eng == "act":
                S.op("act", lambda e, kc=kc, stg_ap=stg_ap: e.copy(out=dst[:, kc, :], in_=stg_ap[:, 0:n_]), r=[skey], w=[key])
            else:
                S.op(eng, lambda e, kc=kc, stg_ap=stg_ap: e.tensor_copy(out=dst[:, kc, :], in_=stg_ap[:, 0:n_]), r=[skey], w=[key])

    def rms_to_bf16(xt_ap, xkey, g_ap, gkey, out_ap, okey, junk, ss, rstd, tag):
        S.op("act", lambda e: e.activation(out=junk[:], in_=xt_ap, func=AF.Square, accum_out=ss[:]),
             r=[xkey], w=["junk" + tag, "ss" + tag])
        S.op("act", lambda e: e.activation(out=rstd[:], in_=ss[:], func=AF.Sqrt, scale=1.0 / D, bias=1e-6),
             r=["ss" + tag], w=["rstd" + tag])
        S.op("dve", lambda e: e.reciprocal(out=rstd[:], in_=rstd[:]), r=["rstd" + tag], w=["rstd" + tag])
        S.op("dve", lambda e: e.scalar_tensor_tensor(out=out_ap, in0=xt_ap, scalar=rstd[:, 0:1], in1=g_ap,
                                                     op0=ALU.mult, op1=ALU.mult),
             r=[xkey, "rstd" + tag, gkey], w=[okey])

    def transpose8(src, skey, dst_ap, dkey, pbank, evac="act"):
        pb = ps[pbank][:].bitcast(BF16)
        for c in range(8):
            S.op("pe", lambda e, c=c: e.transpose(out=pb[:, c * 128:(c + 1) * 128], in_=src[:, c * 128:(c + 1) * 128],
                                                  identity=ident_b[:]),
                 r=[skey, "identb"], w=[PK[pbank]])
        src_v = pb[:, 0:1024].rearrange("p (c n) -> p c n", c=8)
        if evac == "act":
            S.op("act", lambda e: e.copy(out=dst_ap, in_=src_v), r=[PK[pbank]], w=[dkey])
        else:
            S.op("dve", lambda e: e.tensor_copy(out=dst_ap, in_=src_v), r=[PK[pbank]], w=[dkey])

    w_in_b = A("w_in", [128, 8, PROJ], BF16)
    w_sT_f = A("w_sTf", [128, 4, 128], F32)
    w_sT_b = A("w_sTb", [128, 4, 128], BF16)
    NVA = 1024 + 512 + 512 + 16
    vecA = A("vecA", [128, NVA], F32)
    gmix = vecA[:, 0:1024]
    ln_g = vecA[:, 1024:1536]
    ln_b = vecA[:, 1536:2048]
    gate_b = vecA[:, 2048:2064]
    S.dma("sp", lambda e: e.dma_start(out=vecA[:, 0:1024], in_=vec_d[:, VOFF["g_mix"][0]:VOFF["g_mix"][0] + 1024]), w=["vecA"], key="vecA")
    S.dma("sp", lambda e: e.dma_start(out=vecA[:, 1024:2048], in_=vec_d[:, VOFF["ln_g"][0]:VOFF["ln_g"][0] + 1024]), w=["vecA"], key="vecA")
    S.dma("sp", lambda e: e.dma_start(out=vecA[:, 2048:2064], in_=vec_d[:, VOFF["gate_b"][0]:VOFF["gate_b"][0] + 16]), w=["vecA"], key="vecA")
    S.dma("sp", lambda e: e.dma_start(out=w_sT_f[:], in_=w_sT_d), w=["w_sTf"], key="w_sTf")
    S.op("dve", lambda e: e.tensor_copy(out=w_sT_b[:], in_=w_sT_f[:]), r=["w_sTf"], w=["w_sTb"])
    stgA = [A("stgA", [128, PROJ], F32) for _ in range(4)]
    load_w_fast(w_in_b, w_in_d, "w_in", 8, [(stgA[q_], f"stgA{q_}") for q_ in range(4)])
    zt = A("zt", [128, 8192], BF16)
    S.op("pool", lambda e: e.memset(zt[:], 0.0), w=["zt"])
    rows_per = 128 * 8
    zf_todo = list(range(NSLOT // rows_per))

    def zfill_one():
        if zf_todo:
            k = zf_todo.pop(0)
            S.dma("pool", lambda e, k=k: e.dma_start(
                out=xd_d[k * rows_per:(k + 1) * rows_per, :].rearrange("(p r) n -> p (r n)", p=128), in_=zt[:]),
                r=["zt"], w=["xd"], key="zfill")


    xts = [A("xt", [128, D], F32) for _ in range(3)]
    junk = A("junk", [128, D], BF16)
    ssA = A("ss", [128, 1], F32)
    rstdA = A("rstd", [128, 1], F32)
    xns = [A("xn", [128, D], BF16) for _ in range(2)]
    xnTs = [A("xnT", [128, 8, 512], BF16) for _ in range(2)]
    zst = [A("zst", [128, 512], BF16) for _ in range(2)]
    vst = [A("vst", [128, 512], BF16) for _ in range(2)]
    ogst = [A("ogst", [128, 512], F32) for _ in range(2)]
    gus = [A("gu", [128, 512], F32) for _ in range(2)]
    gvs = [A("gv", [128, 512], F32) for _ in range(2)]
    gvt = A("gvt", [128, 512], F32)
    gvn = [A("gvn", [128, 512], BF16) for _ in range(2)]
    ygs = [A("yg", [128, 512], BF16) for _ in range(2)]
    st5 = A("st5", [128, 8], F32)

    def load_x(i):
        S.dma("sp", lambda e: e.dma_start(out=xts[i % 3][:], in_=x_d[i * 128:(i + 1) * 128, :]),
              w=[f"xt{i % 3}"], key=f"xt{i % 3}")

    def front(i):
        seg_, j_ = divmod(i, 4)
        p_ = i % 3
        if i + 2 < NT:
            load_x(i + 2)
        rms_to_bf16(xts[p_][:], f"xt{p_}", gmix, "vecA", xns[i % 2][:], f"xn{i % 2}", junk, ssA, rstdA, "A")
        transpose8(xns[i % 2], f"xn{i % 2}", xnTs[seg_ % 2][:, :, j_ * 128:(j_ + 1) * 128], f"xnT{seg_ % 2}_{j_}", 0, evac="dve")

    load_x(0)
    load_x(1)
    front(0)
    front(1)
    pending = []
    for seg in range(NT // 4):
        sb = seg % 2
        xnT = xnTs[sb]
        for j in range(4):
            i = seg * 4 + j
            p = i % 2
            blocks = [(2, 1024, 512), (3, 1536, 512), (6, 2048, 16), (4, 2064, 512), (5, 2576, 512)]
            for (bank, c0, n) in blocks:
                for kc in range(8):
                    S.op("pe", lambda e, bank=bank, c0=c0, n=n, kc=kc, j=j, xnT=xnT: e.matmul(
                        ps[bank][:, 0:n], lhsT=xnT[:, kc, j * 128:(j + 1) * 128], rhs=w_in_b[:, kc, c0:c0 + n],
                        start=(kc == 0), stop=(kc == 7)),
                        r=[f"xnT{sb}_{j}", "w_in"], w=[PK[bank]])
            if i + 2 < NT:
                front(i + 2)
            S.op("dve", lambda e, p=p: e.tensor_copy(out=vst[p][:], in_=ps[2][:, :]), r=[PK[2]], w=[f"vst{p}"])
            S.dma("pool", lambda e, p=p, i=i: e.dma_start(out=v_d[i * 128:(i + 1) * 128, :], in_=vst[p][:]),
                  r=[f"vst{p}"], w=["v_d"], key=f"vst{p}")
            zfill_one()
            S.op("act", lambda e, p=p: e.activation(out=ogst[p][:], in_=ps[3][:, :], func=AF.Sigmoid),
                 r=[PK[3]], w=[f"ogst{p}"])
            S.dma("pool", lambda e, p=p, i=i: e.dma_start(out=og_d[i * 128:(i + 1) * 128, :], in_=ogst[p][:]),
                  r=[f"ogst{p}"], w=["og_d"], key=f"ogst{p}")
            S.op("dve", lambda e, i=i: e.tensor_tensor(out=gates_all[:, i, :], in0=ps[6][:, 0:16], in1=gate_b, op=ALU.add),
                 r=[PK[6], "vecA"], w=["gates"])
            S.op("act", lambda e, p=p: e.activation(out=gus[p][:], in_=ps[4][:, :], func=AF.Gelu_apprx_tanh),
                 r=[PK[4]], w=[f"gu{p}"])
            S.op("act", lambda e, p=p: e.activation(out=gvs[p][:], in_=ps[5][:, :], func=AF.Gelu_apprx_tanh,
                                                    accum_out=st5[:, 0:1]),
                 r=[PK[5]], w=[f"gv{p}", "st_s1"])
            S.op("act", lambda e, p=p: e.activation(out=junk[:, 0:512], in_=gvs[p][:], func=AF.Square,
                                                    accum_out=st5[:, 1:2]),
                 r=[f"gv{p}"], w=["junkA", "st_s2"])
            S.op("dve", lambda e: e.tensor_scalar(out=st5[:, 2:3], in0=st5[:, 0:1], scalar1=1.0 / 512, scalar2=None,
                                                  op0=ALU.mult), r=["st_s1"], w=["st_m"])
            S.op("dve", lambda e: e.tensor_tensor(out=st5[:, 3:4], in0=st5[:, 2:3], in1=st5[:, 2:3], op=ALU.mult),
                 r=["st_m"], w=["st_msq"])
            S.op("dve", lambda e: e.scalar_tensor_tensor(out=st5[:, 4:5], in0=st5[:, 1:2], scalar=1.0 / 512,
                                                         in1=st5[:, 3:4], op0=ALU.mult, op1=ALU.subtract),
                 r=["st_s2", "st_msq"], w=["st_var"])
            S.op("act", lambda e: e.activation(out=st5[:, 5:6], in_=st5[:, 4:5], func=AF.Sqrt, bias=1e-5),
                 r=["st_var"], w=["st_sd"])
            S.op("dve", lambda e: e.reciprocal(out=st5[:, 6:7], in_=st5[:, 5:6]), r=["st_sd"], w=["st_rs"])
            S.op("dve", lambda e, p=p: e.tensor_scalar(out=gvt[:], in0=gvs[p][:], scalar1=st5[:, 2:3], scalar2=st5[:, 6:7],
                                                       op0=ALU.subtract, op1=ALU.mult),
                 r=[f"gv{p}", "st_m", "st_rs"], w=["gvt"])
            S.op("pool", lambda e: e.tensor_tensor(out=gvt[:], in0=gvt[:], in1=ln_g, op=ALU.mult),
                 r=["gvt", "vecA"], w=["gvt"])
            S.op("pool", lambda e, p=p: e.tensor_tensor(out=gvn[p][:], in0=gvt[:], in1=ln_b, op=ALU.add),
                 r=["gvt", "vecA"], w=[f"gvn{p}"])
            def back2(p=p, i=i):
                for g in range(4):
                    S.op("pe", lambda e, g=g, p=p: e.matmul(ps[7][:, g * 128:(g + 1) * 128], lhsT=w_sT_b[:, g, :],
                                                            rhs=gvn[p][:, g * 128:(g + 1) * 128], start=True, stop=True),
                         r=["w_sTb", f"gvn{p}"], w=[PK[7]])
                for g in range(4):
                    S.op("dve", lambda e, g=g, p=p: e.scalar_tensor_tensor(
                        out=ygs[p][:, g * 128:(g + 1) * 128], in0=ps[7][:, g * 128:(g + 1) * 128],
                        scalar=col[:, 48 + g:49 + g], in1=gus[p][:, g * 128:(g + 1) * 128], op0=ALU.add, op1=ALU.mult),
                        r=[PK[7], "col", f"gu{p}"], w=[f"yg{p}"])
                S.dma("pool", lambda e, p=p, i=i: e.dma_start(out=y_d[i * 128:(i + 1) * 128, 512:1024], in_=ygs[p][:]),
                      r=[f"yg{p}"], w=["y_d"], key=f"yg{p}")
            if pending:
                pending.pop()()
            pending.append(back2)
        for fc in range(8):
            zp = fc % 2
            zb = 1 if fc % 2 == 0 else 6
            for kc in range(8):
                S.op("pe", lambda e, fc=fc, kc=kc, xnT=xnT, zb=zb: e.matmul(
                    ps[zb][:, :], lhsT=w_in_b[:, kc, fc * 128:(fc + 1) * 128], rhs=xnT[:, kc, :],
                    start=(kc == 0), stop=(kc == 7)), r=[f"xnT{sb}_{jx}" for jx in range(4)] + ["w_in"], w=[PK[zb]])
            S.op("dve", lambda e, zp=zp, zb=zb: e.tensor_copy(out=zst[zp][:], in_=ps[zb][:, :]), r=[PK[zb]], w=[f"zst{zp}"])
            S.dma("pool", lambda e, zp=zp, fc=fc, seg=seg: e.dma_start(
                out=zqk_d[fc * 128:(fc + 1) * 128, seg * 512:(seg + 1) * 512], in_=zst[zp][:]),
                r=[f"zst{zp}"], w=["zqk_d"], key=f"zst{zp}")
    while pending:
        pending.pop()()
    while zf_todo:
        zfill_one()
    S.barrier()
    A.reset(PM)
    if stage == 0:
        return nc, S

    ghead = A("ghead", [128, 512], F32)
    S.dma("sp", lambda e: e.dma_start(out=ghead[:], in_=vec_d[:, VOFF["g_head"][0]:VOFF["g_head"][0] + 512]), w=["ghead"], key="ghead")
    zcs = [A("zc", [128, SEQ + 4], BF16) for _ in range(2)]
    diag = A("diag", [128, 8, 5, 128], BF16)
    og_t = A("og_t", [128, NCH, 128], F32)
    hs = A("hs", [128, NCH, 128], F32)
    ybf = A("ybf", [128, NCH, 128], BF16)
    qT = A("qT", [128, SEQ], BF16)
    kT = A("kT", [128, SEQ], BF16)
    vaug = A("vaug", [128, NCH, 129], BF16)
    ktok = A("ktok", [128, NCH, 128], BF16)
    Vp = [A("Vp", [128, NCH, 129], BF16) for _ in range(2)]
    PTa = [A("PTa", [128, NCH, 128], BF16) for _ in range(2)]
    Z = [A("Z", [128, NCH, 129], F32) for _ in range(2)]
    Zb = [A("Zb", [128, NCH + 1, 129], BF16) for _ in range(2)]
    gsm = A("gsm", [128, 16, NCH], F32)
    S.op("pool", lambda e: e.memset(zcs[0][:], 0.0), w=["zc0"])
    S.op("pool", lambda e: e.memset(zcs[1][:], 0.0), w=["zc1"])
    S.op("pool", lambda e: e.memset(vaug[:], 1.0), w=["vaug"])
    for fc_ in range(8):
        for jj_ in range(5):
            S.op("dve", lambda e, fc_=fc_, jj_=jj_: e.tensor_scalar(out=diag[:, fc_, jj_, :], in0=cst[:, 0:128],
                                                                 scalar1=col[:, fc_ * 5 + jj_:fc_ * 5 + jj_ + 1], scalar2=None,
                                                                 op0=ALU.mult), r=["cst", "col"], w=["diag"])
    S.op("pool", lambda e: e.memset(Zb[0][:], 0.0), w=["Zb0"])
    S.op("pool", lambda e: e.memset(Zb[1][:], 0.0), w=["Zb1"])
    if stage == -1:
        S.barrier()
        return nc, S
    masks = [maskf, maskb]
    SCALE = 128 ** -0.5
    import math
    LNS = math.log(SCALE)
    first_bh = True

    for b in range(2):
        for h in range(4):
            for wi, (fc, dst, dkey) in enumerate([(h, qT, "qT"), (4 + h, kT, "kT")]):
                zc = zcs[wi]
                zk = f"zc{wi}"
                S.dma("sp", lambda e, zc=zc, fc=fc, b=b: e.dma_start(
                    out=zc[:, 2:2 + SEQ], in_=zqk_d[fc * 128:(fc + 1) * 128, b * SEQ:(b + 1) * SEQ]),
                    w=[zk], key=zk)
                for cb in range(8):
                    bank = cb % 2
                    for jj in range(5):
                        S.op("pe", lambda e, zc=zc, fc=fc, cb=cb, jj=jj, bank=bank: e.matmul(
                            ps[bank][:, :], lhsT=diag[:, fc, jj, :], rhs=zc[:, cb * 512 + jj:cb * 512 + jj + 512],
                            start=(jj == 0), stop=(jj == 4)), r=["diag", zk], w=[PK[bank]])
                    S.op("act", lambda e, dst=dst, cb=cb, fc=fc, bank=bank: e.activation(
                        out=dst[:, cb * 512:(cb + 1) * 512], in_=ps[bank][:, :], func=AF.Silu, bias=col[:, 40 + fc:41 + fc]),
                        r=[PK[bank], "col"], w=[dkey])
            first_bh = False
            S.dma("sp", lambda e, b=b, h=h: e.dma_start(
                out=vaug[:, :, 0:128], in_=v_d[b * SEQ:(b + 1) * SEQ, h * 128:(h + 1) * 128].rearrange("(c p) d -> p c d", p=128)),
                w=["vaug"], key="vaug")
            S.dma("sp", lambda e, b=b, h=h: e.dma_start(
                out=og_t[:], in_=og_d[b * SEQ:(b + 1) * SEQ, h * 128:(h + 1) * 128].rearrange("(c p) d -> p c d", p=128)),
                w=["og_t"], key="og_t")
            pb6 = ps[6][:].bitcast(BF16)
            for g8 in range(4):
                for cc in range(8):
                    c = g8 * 8 + cc
                    S.op("pe", lambda e, c=c, cc=cc: e.transpose(out=pb6[:, cc * 128:(cc + 1) * 128],
                                                                 in_=kT[:, c * 128:(c + 1) * 128], identity=ident_b[:]),
                         r=["kT", "identb"], w=[PK[6]])
                S.op("act", lambda e, g8=g8: e.copy(out=ktok[:, g8 * 8:(g8 + 1) * 8, :],
                                                    in_=pb6[:, 0:1024].rearrange("p (c n) -> p c n", c=8)),
                     r=[PK[6]], w=["ktok"])
            for d in range(2):
                icol = (0 if d == 0 else 8) + h
                fcol = (4 if d == 0 else 12) + h
                gi = gates_all[:, b * NCH:(b + 1) * NCH, icol]
                gf = gates_all[:, b * NCH:(b + 1) * NCH, fcol]
                S.op("act", lambda e, gf=gf: e.activation(out=gsm[:, 10, :], in_=gf, func=AF.Exp, scale=-1.0),
                     r=["gates"], w=["g_tmp"])
                S.op("act", lambda e, d=d: e.activation(out=gsm[:, d, :], in_=gsm[:, 10, :], func=AF.Ln, bias=1.0),
                     r=["g_tmp"], w=[f"g_spl{d}"])
                S.op("pe", lambda e, d=d: e.matmul(ps[7][:, d * 64:d * 64 + 32], lhsT=masks[d], rhs=gsm[:, d, :],
                                                   start=True, stop=True), r=["cst", f"g_spl{d}"], w=[PK[7]])
                S.op("pe", lambda e, d=d: e.matmul(ps[7][:, d * 64 + 32:d * 64 + 64], lhsT=ones_f, rhs=gsm[:, d, :],
                                                   start=True, stop=True), r=["cst", f"g_spl{d}"], w=[PK[7]])
                S.op("act", lambda e, d=d: e.activation(out=gsm[:, 2 + d, :], in_=ps[7][:, d * 64:d * 64 + 32], func=AF.Exp,
                                                        scale=-1.0, bias=LNS), r=[PK[7]], w=[f"g_rs{d}"])
                S.op("dve", lambda e, d=d, gi=gi: e.tensor_tensor(out=gsm[:, 10, :], in0=ps[7][:, d * 64:d * 64 + 32], in1=gi,
                                                                  op=ALU.add), r=[PK[7], "gates"], w=["g_tmp"])
                S.op("act", lambda e, d=d: e.activation(out=gsm[:, 4 + d, :], in_=gsm[:, 10, :], func=AF.Exp),
                     r=["g_tmp"], w=[f"g_u{d}"])
                S.op("act", lambda e, d=d: e.activation(out=gsm[:, 8 + d, :], in_=ps[7][:, d * 64 + 32:d * 64 + 64],
                                                        func=AF.Exp, scale=-1.0), r=[PK[7]], w=[f"g_eF{d}"])
                S.op("dve" if d == 0 else "pool", lambda e, d=d: e.tensor_tensor(out=Vp[d][:], in0=vaug[:],
                                                            in1=gsm[:, 4 + d, :].unsqueeze(2).to_broadcast([128, NCH, 129]),
                                                            op=ALU.mult), r=["vaug", f"g_u{d}"], w=[f"Vp{d}"])
            for g4 in range(NCH // 4):
                bank = g4 % 2
                for cc in range(4):
                    c = g4 * 4 + cc
                    cs = slice(c * 128, (c + 1) * 128)
                    S.op("pe", lambda e, cs=cs, cc=cc, bank=bank: e.matmul(ps[bank][:, cc * 128:(cc + 1) * 128], lhsT=kT[:, cs],
                                                                           rhs=qT[:, cs], start=True, stop=True),
                         r=["kT", "qT"], w=[PK[bank]])
                for d in range(2):
                    S.op("dve", lambda e, d=d, g4=g4, bank=bank: e.tensor_tensor(
                        out=PTa[d][:, g4 * 4:(g4 + 1) * 4, :], in0=ps[bank][:, :].rearrange("p (c n) -> p c n", c=4),
                        in1=masks[d].unsqueeze(1).to_broadcast([128, 4, 128]), op=ALU.mult),
                        r=[PK[bank], "cst"], w=[f"PTa{d}"])
            nslot = 0
            for c in range(NCH):
                for d in range(2):
                    bank = 2 + nslot % 4
                    nslot += 1
                    S.op("pe", lambda e, d=d, c=c, bank=bank: e.matmul(ps[bank][:, 0:129], lhsT=ktok[:, c, :], rhs=Vp[d][:, c, :],
                                                                       start=True, stop=True), r=["ktok", f"Vp{d}"], w=[PK[bank]])
                    S.op("act", lambda e, d=d, c=c, bank=bank: e.activation(out=Z[d][:, c, :], in_=ps[bank][:, 0:129],
                                                                            func=AF.Identity, scale=gsm[:, 8 + d, c:c + 1]),
                         r=[PK[bank], f"g_eF{d}"], w=[f"Z{d}"])
            for stp in range(1, NCH):
                cf = stp
                cb = NCH - 1 - stp
                S.op("dve", lambda e, cf=cf: e.scalar_tensor_tensor(out=Z[0][:, cf, :], in0=Z[0][:, cf - 1, :],
                                                                    scalar=gsm[:, 8, cf:cf + 1], in1=Z[0][:, cf, :],
                                                                    op0=ALU.mult, op1=ALU.add), r=["Z0", "g_eF0"], w=["Z0"])
                S.op("dve", lambda e, cb=cb: e.scalar_tensor_tensor(out=Z[1][:, cb, :], in0=Z[1][:, cb + 1, :],
                                                                    scalar=gsm[:, 9, cb:cb + 1], in1=Z[1][:, cb, :],
                                                                    op0=ALU.mult, op1=ALU.add), r=["Z1", "g_eF1"], w=["Z1"])
            S.op("dve", lambda e: e.tensor_copy(out=Zb[0][:, 1:NCH, :], in_=Z[0][:, 0:NCH - 1, :]), r=["Z0"], w=["Zb0"])
            S.op("act", lambda e: e.copy(out=Zb[1][:, 1:NCH, :], in_=Z[1][:, 1:NCH, :]), r=["Z1"], w=["Zb1"])
            slot = 0
            for c0 in range(0, NCH, 3):
                ncs = min(3, NCH - c0)
                for d in range(2):
                    bank = 2 + slot % 6
                    slot += 1
                    for cc in range(ncs):
                        c = c0 + cc
                        cs = slice(c * 128, (c + 1) * 128)
                        zi = c if d == 0 else c + 1
                        S.op("pe", lambda e, d=d, c=c, cc=cc, bank=bank: e.matmul(
                            ps[bank][:, cc * 129:(cc + 1) * 129], lhsT=PTa[d][:, c, :], rhs=Vp[d][:, c, :], start=True, stop=False),
                            r=[f"PTa{d}", f"Vp{d}"], w=[PK[bank]])
                        S.op("pe", lambda e, d=d, cs=cs, cc=cc, bank=bank, zi=zi: e.matmul(
                            ps[bank][:, cc * 129:(cc + 1) * 129], lhsT=qT[:, cs], rhs=Zb[d][:, zi, :], start=False, stop=True),
                            r=["qT", f"Zb{d}"], w=[PK[bank]])
                    src = ps[bank][:, 0:ncs * 129].rearrange("p (c n) -> p c n", c=ncs)
                    if slot % 2 == 0:
                        S.op("act", lambda e, d=d, c0=c0, ncs=ncs, src=src: e.copy(out=Z[d][:, c0:c0 + ncs, :], in_=src),
                             r=[PK[bank], f"Zb{d}"], w=[f"Z{d}"])
                    else:
                        S.op("dve", lambda e, d=d, c0=c0, ncs=ncs, src=src: e.tensor_copy(out=Z[d][:, c0:c0 + ncs, :], in_=src),
                             r=[PK[bank], f"Zb{d}"], w=[f"Z{d}"])
            hraw = Z
            for d in range(2):
                den = hraw[d][:, :, 128]
                S.op("dve", lambda e, d=d, den=den: e.tensor_tensor(out=gsm[:, 12 + d, :], in0=den, in1=gsm[:, 2 + d, :],
                                                                   op=ALU.mult), r=[f"Z{d}", f"g_rs{d}"], w=[f"g_cf{d}"])
                S.op("dve", lambda e, d=d: e.tensor_scalar(out=gsm[:, 15, :], in0=gsm[:, 12 + d, :], scalar1=-1.0,
                                                           scalar2=1.0, op0=ALU.mult, op1=ALU.max),
                     r=[f"g_cf{d}"], w=["g_neg"])
                S.op("dve", lambda e, d=d: e.tensor_tensor(out=gsm[:, 12 + d, :], in0=gsm[:, 12 + d, :], in1=gsm[:, 15, :],
                                                           op=ALU.max), r=[f"g_cf{d}", "g_neg"], w=[f"g_cf{d}"])
                S.op("dve", lambda e, d=d: e.reciprocal(out=gsm[:, 12 + d, :], in_=gsm[:, 12 + d, :]),
                     r=[f"g_cf{d}"], w=[f"g_cf{d}"])
                S.op("dve", lambda e, d=d: e.tensor_tensor(out=gsm[:, 12 + d, :], in0=gsm[:, 12 + d, :], in1=gsm[:, 2 + d, :],
                                                           op=ALU.mult), r=[f"g_cf{d}", f"g_rs{d}"], w=[f"g_cf{d}"])
            bc = lambda ap: ap.unsqueeze(2).to_broadcast([128, NCH, 128])
            z1v = Z[1][:, :, 0:128]
            S.op("dve", lambda e: e.tensor_tensor(out=hs[:], in0=Z[0][:, :, 0:128], in1=bc(gsm[:, 12, :]), op=ALU.mult),
                 r=["Z0", "g_cf0"], w=["hs"])
            S.op("pool", lambda e: e.tensor_tensor(out=z1v, in0=z1v, in1=bc(gsm[:, 13, :]), op=ALU.mult),
                 r=["Z1", "g_cf1"], w=["Z1"])
            S.op("dve", lambda e: e.tensor_tensor(out=hs[:], in0=hs[:], in1=z1v, op=ALU.add), r=["hs", "Z1"], w=["hs"])
            S.op("dve", lambda e: e.tensor_tensor(out=z1v, in0=hs[:], in1=hs[:], op=ALU.mult), r=["hs", "Z1"], w=["Z1"])
            S.op("dve", lambda e: e.tensor_reduce(out=gsm[:, 14, :], in_=z1v, axis=AX.X, op=ALU.add),
                 r=["Z1"], w=["g_ss"])
            S.op("act", lambda e: e.activation(out=gsm[:, 14, :], in_=gsm[:, 14, :], func=AF.Sqrt, scale=1.0 / 128, bias=1e-6),
                 r=["g_ss"], w=["g_ss"])
            S.op("dve", lambda e: e.reciprocal(out=gsm[:, 14, :], in_=gsm[:, 14, :]), r=["g_ss"], w=["g_ss"])
            S.op("dve", lambda e: e.tensor_tensor(out=hs[:], in0=hs[:], in1=bc(gsm[:, 14, :]), op=ALU.mult),
                 r=["hs", "g_ss"], w=["hs"])
            S.op("dve", lambda e, h=h: e.tensor_tensor(out=hs[:], in0=hs[:],
                                                        in1=ghead[:, h * 128:(h + 1) * 128].unsqueeze(1).to_broadcast([128, NCH, 128]),
                                                        op=ALU.mult), r=["hs", "ghead"], w=["hs"])
            S.op("dve", lambda e: e.tensor_tensor(out=ybf[:], in0=hs[:], in1=og_t[:], op=ALU.mult),
                 r=["hs", "og_t"], w=["ybf"])
            S.dma("pool", lambda e, b=b, h=h: e.dma_start(
                out=y_d[b * SEQ:(b + 1) * SEQ, h * 128:(h + 1) * 128].rearrange("(c p) d -> p c d", p=128), in_=ybf[:]),
                r=["ybf"], w=["y_d"], key="ybf")
    S.barrier()
    A.reset(PM)
    if stage == -2:
        return nc, S
    w_out_b = A("w_out", [128, 8, D], BF16)
    w_q_b = A("w_q", [128, 8, D], BF16)
    w_o_b = A("w_o", [128, 8, D], BF16)
    w_r_b = A("w_r", [128, 8, 36], BF16)
    vecB = A("vecB", [128, 3 * 1024 + 36 + 32], F32)
    gx = vecB[:, 0:1024]
    gmem = vecB[:, 1024:2048]
    gmoe = vecB[:, 2048:3072]
    b_r = vecB[:, 3072:3108]
    iota = vecB[:, 3108:3140]
    S.dma("sp", lambda e: e.dma_start(out=vecB[:, 0:3072], in_=vec_d[:, VOFF["g_xattn"][0]:VOFF["g_xattn"][0] + 3072]), w=["vecB"], key="vecB")
    S.dma("sp", lambda e: e.dma_start(out=vecB[:, 3072:3140], in_=vec_d[:, VOFF["b_r"][0]:VOFF["b_r"][0] + 68]), w=["vecB"], key="vecB")
    load_w(w_r_b, w_r_d, "w_r", 8)

    xtsB = [A("xtB", [128, D], F32) for _ in range(2)]
    yts = [A("ytB", [128, D], BF16) for _ in range(2)]
    yT = A("yT", [128, 8, 128], BF16)
    x1 = A("x1", [128, 4, D], F32)
    junkB = A("junkB", [128, D], BF16)
    ssB = A("ssB", [128, 1], F32)
    rstdB = A("rstdB", [128, 1], F32)
    xn1 = A("xn1", [128, D], BF16)
    xn1T = A("xn1T", [128, 8, 512], BF16)
    qTx = A("qTx", [128, 8, 512], BF16)
    hmT = A("hmT", [128, 8, 256], BF16)
    kmT = [A("kmT", [128, 8, 256], BF16) for _ in range(2)]
    vm = [A("vm", [128, 2, D], BF16) for _ in range(2)]
    pT = [A("pT", [128, 512], BF16) for _ in range(2)]
    rden = A("rden", [128, 512], F32)
    ocT = A("ocT", [128, 8, 512], BF16)
    x2t = [A("x2t", [128, D], F32) for _ in range(2)]
    hbs = [A("hb", [128, D], BF16) for _ in range(4)]
    yT_c = A("yT", [128, 8, 128], BF16)
    yT_d = A("yT", [128, 8, 128], BF16)
    ss4 = A("ss4", [128, 4], F32)
    rstd4 = A("rstd4", [128, 4], F32)
    run_bc = A("run_bc", [128, 32], F32)
    S.op("pool", lambda e: e.memset(run_bc[:], 0.0), w=["run_bc"])
    KVM = A.mark()
    w_kv_b = A("w_kv", [128, 8, 2 * D], BF16)
    stgB = [(x1[:, 0:2, :].rearrange("p a n -> p (a n)"), "stgB0"), (x1[:, 2:4, :].rearrange("p a n -> p (a n)"), "stgB1"),
            (ocT[:].rearrange("p a n -> p (a n)").bitcast(F32), "stgB2"), (qTx[:].rearrange("p a n -> p (a n)").bitcast(F32), "stgB3"),
            (xn1T[:].rearrange("p a n -> p (a n)").bitcast(F32), "stgB4")]
    load_w_fast(w_kv_b, w_kv_d, "w_kv", 8, stgB)

    for b in range(2):
        for mt in range(2):
            p = mt
            S.dma("sp", lambda e, b=b, mt=mt, p=p: e.dma_start(out=xtsB[p][:], in_=mem_d[b * 256 + mt * 128: b * 256 + (mt + 1) * 128, :]),
                  w=[f"xtB{p}"], key=f"xtB{p}")
            rms_to_bf16(xtsB[p][:], f"xtB{p}", gmem, "vecB", xn1[:], "xn1", junkB, ssB, rstdB, "B")
            transpose8(xn1, "xn1", hmT[:, :, mt * 128:(mt + 1) * 128], "hmT", 0)
        for fc in range(8):
            bank = 1 + fc % 2
            for kc in range(8):
                S.op("pe", lambda e, fc=fc, kc=kc, bank=bank: e.matmul(
                    ps[bank][:, 0:256], lhsT=w_kv_b[:, kc, fc * 128:(fc + 1) * 128], rhs=hmT[:, kc, :],
                    start=(kc == 0), stop=(kc == 7)), r=["w_kv", "hmT"], w=[PK[bank]])
            S.op("act", lambda e, fc=fc, b=b, bank=bank: e.copy(out=kmT[b][:, fc, :], in_=ps[bank][:, 0:256]),
                 r=[PK[bank]], w=[f"kmT{b}"])
        for mt in range(2):
            for half in range(2):
                bank = 3 + half
                for kc in range(8):
                    S.op("pe", lambda e, mt=mt, half=half, kc=kc, bank=bank: e.matmul(
                        ps[bank][:, :], lhsT=hmT[:, kc, mt * 128:(mt + 1) * 128],
                        rhs=w_kv_b[:, kc, 1024 + half * 512:1024 + (half + 1) * 512],
                        start=(kc == 0), stop=(kc == 7)), r=["w_kv", "hmT"], w=[PK[bank]])
                S.op("dve", lambda e, mt=mt, half=half, b=b, bank=bank: e.tensor_copy(
                    out=vm[b][:, mt, half * 512:(half + 1) * 512], in_=ps[bank][:, :]), r=[PK[bank]], w=[f"vm{b}"])

    load_w_fast(w_out_b, w_out_d, "w_out", 8, stgB)
    load_w_fast(w_q_b, w_q_d, "w_q", 8, stgB)
    load_w_fast(w_o_b, w_o_d, "w_o", 8, stgB)
    XSC = 256 ** -0.5
    S.barrier()
    A.reset(KVM)
    xtsB3 = xtsB + [A("xtB", [128, D], F32)]
    yts3 = yts + [A("ytB", [128, D], BF16)]
    yT2 = [yT, A("yT", [128, 8, 128], BF16)]
    yT4 = yT2 + [yT_c, yT_d]
    xn1s = [xn1, A("xn1", [128, D], BF16)]
    x2t4 = x2t + [A("x2t", [128, D], F32) for _ in range(2)]
    hbs8 = hbs + [A("hb", [128, D], BF16) for _ in range(4)]
    KVM_R = A.mark()

    def load_xy_x(i):
        p = i % 3
        S.dma("sp", lambda e: e.dma_start(out=xtsB3[p][:], in_=x_d[i * 128:(i + 1) * 128, :]), w=[f"xtB{p}"], key=f"xtB{p}")

    def load_xy_y(i):
        p = i % 3
        S.dma("sp", lambda e: e.dma_start(out=yts3[p][:], in_=y_d[i * 128:(i + 1) * 128, :]), w=[f"ytB{p}"], key=f"ytB{p}")

    def load_xy(i):
        load_xy_x(i)
        load_xy_y(i)

    hT2 = yT2

    A.reset(KVM_R)
    rsegs = [A("rseg", [128, 4, 288], F32) for _ in range(2)]
    rAbs = [A("rAbs", [128, 4, 32], BF16) for _ in range(2)]

    def router_chain(seg):
        q2 = seg % 2
        rs = rsegs[q2]
        R = [f"rseg{q2}"]
        V = lambda a, b_: rs[:, :, a:b_]
        S.op("dve", lambda e: e.tensor_tensor(out=V(0, 36), in0=ps[5][:, 256:400].rearrange("p (t n) -> p t n", t=4),
                                              in1=b_r.unsqueeze(1).to_broadcast([128, 4, 36]), op=ALU.add),
             r=["ps5r0", "ps5r1", "ps5r2", "ps5r3", "vecB"], w=R)
        S.op("dve", lambda e: e.tensor_reduce(out=rs[:, :, 40], in_=V(0, 4), axis=AX.X, op=ALU.max), r=R, w=R)
        S.op("dve", lambda e: e.tensor_tensor(out=V(52, 56), in0=V(0, 4), in1=V(40, 41).to_broadcast([128, 4, 4]), op=ALU.subtract), r=R, w=R)
        S.op("act", lambda e: e.activation(out=V(52, 56), in_=V(52, 56), func=AF.Exp), r=R, w=R)
        S.op("dve", lambda e: e.tensor_reduce(out=rs[:, :, 42], in_=V(52, 56), axis=AX.X, op=ALU.add), r=R, w=R)
        S.op("dve", lambda e: e.reciprocal(out=V(43, 44), in_=V(42, 43)), r=R, w=R)
        S.op("dve", lambda e: e.tensor_tensor(out=V(44, 48), in0=V(0, 4), in1=V(40, 41).to_broadcast([128, 4, 4]), op=ALU.is_equal), r=R, w=R)
        S.op("dve", lambda e: e.tensor_scalar(out=V(48, 52), in0=V(44, 48), scalar1=1e9, scalar2=-1e9, op0=ALU.mult, op1=ALU.add), r=R, w=R)
        S.op("dve", lambda e: e.tensor_tensor(out=V(56, 88).rearrange("p t (g k) -> p t g k", g=4),
                                              in0=V(4, 36).rearrange("p t (g k) -> p t g k", g=4),
                                              in1=V(48, 52).unsqueeze(3).to_broadcast([128, 4, 4, 8]), op=ALU.add), r=R, w=R)
        for t in range(4):
            S.op("dve", lambda e, t=t: e.max(out=rs[:, t, 88:96], in_=rs[:, t, 56:88]), r=R, w=R)
            S.op("dve", lambda e, t=t: e.max_index(out=rs[:, t, 96:104].bitcast(U32), in_max=rs[:, t, 88:96], in_values=rs[:, t, 56:88]), r=R, w=R)
        for t in range(4):
            S.op("dve", lambda e, t=t: e.tensor_copy(out=rs[:, t, 104:106], in_=rs[:, t, 96:98].bitcast(U32)), r=R, w=R)
        S.op("dve", lambda e: e.tensor_tensor(out=V(106, 107), in0=V(88, 89), in1=V(89, 90), op=ALU.subtract), r=R, w=R)
        S.op("act", lambda e: e.activation(out=V(107, 108), in_=V(106, 107), func=AF.Sigmoid), r=R, w=R)
        S.op("dve", lambda e: e.tensor_tensor(out=V(108, 109), in0=V(107, 108), in1=V(43, 44), op=ALU.mult), r=R, w=R)
        S.op("dve", lambda e: e.tensor_tensor(out=V(109, 110), in0=V(43, 44), in1=V(108, 109), op=ALU.subtract), r=R, w=R)
        for k in range(2):
            S.op("dve", lambda e, k=k: e.tensor_tensor(out=V(112 + 32 * k, 144 + 32 * k), in0=iota.unsqueeze(1).to_broadcast([128, 4, 32]),
                                                       in1=V(104 + k, 105 + k).to_broadcast([128, 4, 32]), op=ALU.is_equal),
                 r=R + ["vecB"], w=R)
        S.op("dve", lambda e: e.tensor_tensor(out=rAbs[q2][:], in0=V(112, 144), in1=V(144, 176), op=ALU.add), r=R, w=R)

    def router_tail_seg(seg):
        q2 = seg % 2
        rs = rsegs[q2]
        R = [f"rseg{q2}"]
        V = lambda a, b_: rs[:, :, a:b_]
        Ab = rAbs[q2]
        for t in range(4):
            S.op("pe", lambda e, t=t: e.matmul(ps[5][:, 32 * t:32 * t + 32], lhsT=strict_b[:], rhs=Ab[:, t, :], start=True, stop=(t == 0)),
                 r=["strictb"] + R, w=["ps5k"])
            for t2 in range(t):
                S.op("pe", lambda e, t=t, t2=t2: e.matmul(ps[5][:, 32 * t:32 * t + 32], lhsT=ones_b[:], rhs=Ab[:, t2, :],
                                                          start=False, stop=(t2 == t - 1)), r=["onesb"] + R, w=["ps5k"])
        for t in range(4):
            S.op("pe", lambda e, t=t: e.matmul(ps[5][:, 128:160], lhsT=ones_b[:], rhs=Ab[:, t, :], start=(t == 0), stop=(t == 3)),
                 r=["onesb"] + R, w=["ps5k"])
        S.op("dve", lambda e: e.tensor_tensor(out=V(208, 240), in0=ps[5][:, 0:128].rearrange("p (t n) -> p t n", t=4),
                                              in1=run_bc[:].unsqueeze(1).to_broadcast([128, 4, 32]), op=ALU.add),
             r=["ps5k", "run_bc"], w=R)
        S.op("dve", lambda e: e.tensor_tensor(out=run_bc[:], in0=ps[5][:, 128:160], in1=run_bc[:], op=ALU.add),
             r=["ps5k", "run_bc"], w=["run_bc"])
        t0 = seg * 4
        for k in range(2):
            S.op("dve", lambda e, k=k: e.tensor_tensor(out=V(240, 272), in0=V(112 + 32 * k, 144 + 32 * k), in1=V(208, 240), op=ALU.mult), r=R, w=R)
            S.op("dve", lambda e, k=k: e.tensor_reduce(out=rs[:, :, 272 + k], in_=V(240, 272), axis=AX.X, op=ALU.add), r=R, w=R)
            S.op("dve", lambda e, k=k: e.scalar_tensor_tensor(out=V(274 + k, 275 + k), in0=V(104 + k, 105 + k), scalar=float(CAP),
                                                              in1=V(272 + k, 273 + k), op0=ALU.mult, op1=ALU.add), r=R, w=R)
            S.op("dve", lambda e, k=k: e.tensor_single_scalar(out=V(276 + k, 277 + k), in_=V(272 + k, 273 + k), scalar=float(CAP),
                                                              op=ALU.is_ge), r=R, w=R)
            S.op("dve", lambda e, k=k: e.scalar_tensor_tensor(out=V(274 + k, 275 + k), in0=V(276 + k, 277 + k), scalar=1.0e6,
                                                              in1=V(274 + k, 275 + k), op0=ALU.mult, op1=ALU.add), r=R, w=R)
            S.op("dve", lambda e, k=k: e.tensor_tensor(out=V(278 + k, 279 + k), in0=V(108 + k, 109 + k), in1=V(276 + k, 277 + k),
                                                       op=ALU.mult), r=R, w=R)
            S.op("dve", lambda e, k=k: e.tensor_tensor(out=wts[:, t0:t0 + 4, k:k + 1], in0=V(108 + k, 109 + k), in1=V(278 + k, 279 + k),
                                                       op=ALU.subtract), r=R, w=["wts"])
        S.op("dve", lambda e: e.tensor_copy(out=slots_i[:, t0:t0 + 4, :], in_=V(274, 276)), r=R, w=["slots"])
        for t in range(4):
            i = t0 + t
            hq = i % 8
            for k in range(2):
                S.dma("pool", lambda e, k=k, i=i, hq=hq: e.indirect_dma_start(
                    out=xd_d[:, :], out_offset=bass.IndirectOffsetOnAxis(ap=slots_i[:, i, k:k + 1], axis=0),
                    in_=hbs8[hq][:, :], in_offset=None, bounds_check=_bcreg(e), oob_is_err=False),
                    r=[f"hb{hq}", "slots"], w=["xd", "scat"], key=f"sc{hq}")

    TB = (0, 6)

    def rms4_stats(srcs):
        for j_, (ap_, k_) in enumerate(srcs):
            S.op("act", lambda e, ap_=ap_, j_=j_: e.activation(out=junkB[:], in_=ap_, func=AF.Square, accum_out=ss4[:, j_:j_ + 1]),
                 r=[k_], w=["junkB_", f"ss4_{j_}"])
        S.op("act", lambda e: e.activation(out=rstd4[:], in_=ss4[:], func=AF.Sqrt, scale=1.0 / D, bias=1e-6),
             r=[f"ss4_{j_}" for j_ in range(4)], w=["rstd4"])
        S.op("dve", lambda e: e.reciprocal(out=rstd4[:], in_=rstd4[:]), r=["rstd4"], w=["rstd4"])

    def rms4_apply(j_, src, g_ap, dst):
        (ap_, k_), (dap_, dk_) = src, dst
        S.op("dve", lambda e: e.scalar_tensor_tensor(out=dap_, in0=ap_, scalar=rstd4[:, j_:j_ + 1], in1=g_ap,
                                                     op0=ALU.mult, op1=ALU.mult), r=[k_, "rstd4", "vecB"], w=[dk_])

    pend_chain = []
    pend_tail = []
    load_xy(0)
    load_xy(1)
    load_xy(2)
    for seg in range(NT // 4):
        b = seg // 8
        for j in range(4):
            i = seg * 4 + j
            p = i % 3
            transpose8(yts3[p], f"ytB{p}", yT4[j][:], f"yT{j}", TB[j % 2], evac="act")
            if i + 3 < NT:
                load_xy_y(i + 3)
        for j in range(4):
            i = seg * 4 + j
            p = i % 3
            for half in range(2):
                bank = (1 if j % 2 == 0 else 3) + half
                for kc in range(8):
                    S.op("pe", lambda e, half=half, kc=kc, bank=bank, j=j: e.matmul(
                        ps[bank][:, :], lhsT=yT4[j][:, kc, :], rhs=w_out_b[:, kc, half * 512:(half + 1) * 512],
                        start=(kc == 0), stop=(kc == 7)), r=[f"yT{j}", "w_out"], w=[PK[bank]])
                S.op("dve", lambda e, half=half, j=j, p=p, bank=bank: e.tensor_tensor(
                    out=x1[:, j, half * 512:(half + 1) * 512], in0=ps[bank][:, :], in1=xtsB3[p][:, half * 512:(half + 1) * 512],
                    op=ALU.add), r=[PK[bank], f"xtB{p}"], w=[f"x1_{j}"])
            if i + 3 < NT:
                load_xy_x(i + 3)
            if stage == 1:
                S.dma("pool", lambda e, i=i, j=j: e.dma_start(out=out_d[i * 128:(i + 1) * 128, :], in_=x1[:, j, :]),
                      r=[f"x1_{j}"], w=[f"out{i}"], key=f"x1o{j}")
            if j == 0:
                while pend_chain:
                    pend_chain.pop(0)()
        if stage == 1:
            continue
        rms4_stats([(x1[:, j, :], f"x1_{j}") for j in range(4)])
        for jp in range(2):
            for j in (2 * jp, 2 * jp + 1):
                rms4_apply(j, (x1[:, j, :], f"x1_{j}"), gx, (xn1s[j % 2][:], f"xn1_{j % 2}"))
            for j in (2 * jp, 2 * jp + 1):
                transpose8(xn1s[j % 2], f"xn1_{j % 2}", xn1T[:, :, j * 128:(j + 1) * 128], f"xn1T_{j}", TB[j % 2],
                           evac=("act" if j % 2 == 0 else "dve"))
        while pend_tail:
            pend_tail.pop(0)()
        for fc in range(8):
            bank = 1 + fc % 2
            for kc in range(8):
                S.op("pe", lambda e, fc=fc, kc=kc, bank=bank: e.matmul(
                    ps[bank][:, :], lhsT=w_q_b[:, kc, fc * 128:(fc + 1) * 128], rhs=xn1T[:, kc, :],
                    start=(kc == 0), stop=(kc == 7)), r=[f"xn1T_{jx}" for jx in range(4)] + ["w_q"], w=[PK[bank]])
            S.op("act", lambda e, fc=fc, bank=bank: e.copy(out=qTx[:, fc, :], in_=ps[bank][:, :]), r=[PK[bank]], w=["qTx"])
        for h in range(4):
            for mt in range(2):
                bank = 3 + mt
                for dc in range(2):
                    S.op("pe", lambda e, h=h, mt=mt, dc=dc, bank=bank, b=b: e.matmul(
                        ps[bank][:, :], lhsT=kmT[b][:, h * 2 + dc, mt * 128:(mt + 1) * 128], rhs=qTx[:, h * 2 + dc, :],
                        start=(dc == 0), stop=(dc == 1)), r=[f"kmT{b}", "qTx"], w=[PK[bank]])
                S.op("act", lambda e, mt=mt, bank=bank: e.activation(out=pT[mt][:], in_=ps[bank][:, :], func=AF.Exp, scale=XSC),
                     r=[PK[bank]], w=[f"pT{mt}"])
            for mt in range(2):
                S.op("pe", lambda e, mt=mt: e.matmul(ps[5][:, :], lhsT=ones_b[:], rhs=pT[mt][:], start=(mt == 0), stop=(mt == 1)),
                     r=["onesb", f"pT{mt}"], w=["ps5r0", "ps5r1", "ps5r2", "ps5r3", "ps5k"])
            S.op("dve", lambda e: e.reciprocal(out=rden[:], in_=ps[5][:, :]), r=["ps5r0", "ps5k"], w=["rden"])
            for ec in range(2):
                bank = 6 + ec
                for mt in range(2):
                    S.op("pe", lambda e, h=h, ec=ec, mt=mt, bank=bank, b=b: e.matmul(
                        ps[bank][:, :], lhsT=vm[b][:, mt, h * 256 + ec * 128:h * 256 + (ec + 1) * 128], rhs=pT[mt][:],
                        start=(mt == 0), stop=(mt == 1)), r=[f"vm{b}", f"pT{mt}"], w=[PK[bank]])
                S.op("dve", lambda e, h=h, ec=ec, bank=bank: e.tensor_tensor(out=ocT[:, h * 2 + ec, :], in0=ps[bank][:, :],
                                                                             in1=rden[:], op=ALU.mult),
                     r=[PK[bank], "rden"], w=["ocT"])
        for j in range(4):
            i = seg * 4 + j
            p4 = i % 4
            for half in range(2):
                bank = 1 + half
                for kc in range(8):
                    S.op("pe", lambda e, half=half, kc=kc, bank=bank, j=j: e.matmul(
                        ps[bank][:, :], lhsT=ocT[:, kc, j * 128:(j + 1) * 128], rhs=w_o_b[:, kc, half * 512:(half + 1) * 512],
                        start=(kc == 0), stop=(kc == 7)), r=["ocT", "w_o"], w=[PK[bank]])
                S.op("dve", lambda e, half=half, j=j, p4=p4, bank=bank: e.tensor_tensor(
                    out=x2t4[p4][:, half * 512:(half + 1) * 512], in0=ps[bank][:, :], in1=x1[:, j, half * 512:(half + 1) * 512],
                    op=ALU.add), r=[PK[bank], f"x1_{j}"], w=[f"x2t{p4}"])
            S.dma("sp", lambda e, i=i, p4=p4: e.dma_start(out=out_d[i * 128:(i + 1) * 128, :], in_=x2t4[p4][:]),
                  r=[f"x2t{p4}"], w=[f"out{i}"], key=f"x2t{p4}")
        if stage == 2:
            continue
        rms4_stats([(x2t4[(seg * 4 + j) % 4][:], f"x2t{(seg * 4 + j) % 4}") for j in range(4)])
        for j in range(4):
            rms4_apply(j, (x2t4[(seg * 4 + j) % 4][:], f"x2t{(seg * 4 + j) % 4}"), gmoe,
                       (hbs8[(seg * 4 + j) % 8][:], f"hb{(seg * 4 + j) % 8}"))
        for j in range(4):
            i = seg * 4 + j
            hq = i % 8
            transpose8(hbs8[hq], f"hb{hq}", yT4[j][:], f"yT{j}", TB[j % 2], evac=("act" if j % 2 == 0 else "dve"))
            for kc in range(8):
                S.op("pe", lambda e, kc=kc, j=j: e.matmul(ps[5][:, 256 + 36 * j:292 + 36 * j], lhsT=yT4[j][:, kc, :], rhs=w_r_b[:, kc, :],
                                                          start=(kc == 0), stop=(kc == 7)), r=[f"yT{j}", "w_r"], w=[f"ps5r{j}"])
        pend_chain.append(lambda seg=seg: router_chain(seg))
        pend_tail.append(lambda seg=seg: router_tail_seg(seg))
    while pend_chain:
        pend_chain.pop(0)()
    while pend_tail:
        pend_tail.pop(0)()
    if stage >= 2.5:
        S.op("dve", lambda e: e.tensor_copy(out=cnt_i[:], in_=run_bc[:]), r=["run_bc"], w=["cnt_i"])
    if debug and stage >= 2.5:
        ds_d = nc.dram_tensor("dbg_slots", [128, NT * 2], I32, kind="ExternalOutput").ap()
        dw_d = nc.dram_tensor("dbg_wts", [128, NT * 2], F32, kind="ExternalOutput").ap()
        S.dma("sp", lambda e: e.dma_start(out=ds_d, in_=slots_i[:].rearrange("p t k -> p (t k)")), r=["slots"], w=["dbgs"], key="dbgs")
        S.dma("sp", lambda e: e.dma_start(out=dw_d, in_=wts[:].rearrange("p t k -> p (t k)")), r=["wts"], w=["dbgw"], key="dbgw")
    S.barrier()
    A.reset(PM)
    if stage < 2.7:
        return nc, S

    wg = [A("wg", [128, 8, 512], BF16) for _ in range(2)]
    wu = [A("wu", [128, 8, 512], BF16) for _ in range(2)]
    wd = [A("wd", [128, 4, D], BF16) for _ in range(2)]
    xbs = [A("xb", [128, D], BF16) for _ in range(6)]
    xbT = [A("xbT", [128, 8, 512], BF16) for _ in range(2)]
    sgt = [A("sgt", [128, 512], F32) for _ in range(2)]
    hmid = [A("hmid", [128, 4, 512], BF16) for _ in range(2)]
    yst = [A("yst", [128, D], F32) for _ in range(3)]

    stg_g = [A("stg_g", [128, 8, 512], F32) for _ in range(2)]
    stg_u = [A("stg_u", [128, 8, 512], F32) for _ in range(2)]
    stg_d = [A("stg_d", [128, 4, D], F32) for _ in range(2)]

    def load_expert_dma(e_):
        q = e_ % 2
        S.dma("act", lambda e: e.dma_start(out=stg_g[q][:], in_=w_gate_d[e_].rearrange("(c p) n -> p c n", p=128)),
              w=[f"stg_g{q}"], key=f"stg_g{q}")
        S.dma("act", lambda e: e.dma_start(out=stg_u[q][:], in_=w_up_d[e_].rearrange("(c p) n -> p c n", p=128)),
              w=[f"stg_u{q}"], key=f"stg_u{q}")
        S.dma("act", lambda e: e.dma_start(out=stg_d[q][:], in_=w_down_d[e_].rearrange("(c p) n -> p c n", p=128)),
              w=[f"stg_d{q}"], key=f"stg_d{q}")

    def load_expert_cast(e_):
        q = e_ % 2
        for hh in range(2):
            S.op("dve", lambda e, hh=hh: e.tensor_copy(out=wg[q][:, hh * 4:(hh + 1) * 4, :], in_=stg_g[q][:, hh * 4:(hh + 1) * 4, :]),
                 r=[f"stg_g{q}"], w=[f"wg{q}"])
            S.op("act", lambda e, hh=hh: e.copy(out=wu[q][:, hh * 4:(hh + 1) * 4, :], in_=stg_u[q][:, hh * 4:(hh + 1) * 4, :]),
                 r=[f"stg_u{q}"], w=[f"wu{q}"])
            S.op("pool", lambda e, hh=hh: e.tensor_copy(out=wd[q][:, hh * 2:(hh + 1) * 2, :], in_=stg_d[q][:, hh * 2:(hh + 1) * 2, :]),
                 r=[f"stg_d{q}"], w=[f"wd{q}"])

    groups = []
    off = 0
    while off < CAP:
        n = min(512 if off == 0 else 256, CAP - off)
        groups.append((off, n))
        off += n
    allg = [(ex_, goff_, n_) for ex_ in range(32) for (goff_, n_) in groups]
    SEC_START = tuple(g_[0] for g_ in groups[1:])
    SEC_END = SEC_START
    NXB = 6
    xb_of = {}
    xbi = 0

    def issue_loads(k):
        nonlocal xbi
        ex_, goff_, n_ = allg[k]
        ids = []
        for blk_ in range(n_ // 128):
            xq_ = xbi % NXB
            xbi += 1
            r0_ = ex_ * CAP + goff_ + blk_ * 128
            S.dma("sp", lambda e, xq_=xq_, r0_=r0_: e.dma_start(out=xbs[xq_][:], in_=xd_d[r0_:r0_ + 128, :]),
                  w=[f"xb{xq_}"], key=f"xb{xq_}")
            ids.append(xq_)
        xb_of[k] = ids

    load_expert_dma(0)
    load_expert_dma(1)
    load_expert_cast(0)
    issue_loads(0)
    gi_ = 0
    ysi = 0
    for k, (ex, goff, n) in enumerate(allg):
        q = ex % 2
        if k + 1 < len(allg):
            issue_loads(k + 1)
        if goff == groups[1][0]:
            if ex + 1 < 32:
                load_expert_cast(ex + 1)
            if ex + 2 < 32:
                load_expert_dma(ex + 2)
        if goff in SEC_START and DYN_SKIP:
            S.section_begin(cnt_i[0:1, ex:ex + 1], goff, tag=ex)
        if True:
            gq = gi_ % 2
            gi_ += 1
            nb = n // 128
            for blk in range(nb):
                xq = xb_of[k][blk]
                transpose8(xbs[xq], f"xb{xq}", xbT[gq][:, :, blk * 128:(blk + 1) * 128], f"xbT{gq}", blk % 2,
                           evac=("act" if blk % 2 == 0 else "dve"))
            for fc in range(4):
                for (wt, wkey, bank) in ((wg[q], f"wg{q}", 2 + (fc % 2)), (wu[q], f"wu{q}", 4 + (fc % 2))):
                    for kc in range(8):
                        S.op("pe", lambda e, wt=wt, fc=fc, kc=kc, bank=bank, gq=gq, n=n: e.matmul(
                            ps[bank][:, 0:n], lhsT=wt[:, kc, fc * 128:(fc + 1) * 128], rhs=xbT[gq][:, kc, 0:n],
                            start=(kc == 0), stop=(kc == 7)), r=[wkey, f"xbT{gq}"], w=[PK[bank]])
                sq = fc % 2
                S.op("act", lambda e, fc=fc, sq=sq, n=n: e.activation(out=sgt[sq][:, 0:n], in_=ps[2 + (fc % 2)][:, 0:n], func=AF.Silu),
                     r=[PK[2 + (fc % 2)]], w=[f"sgt{sq}"])
                S.op("dve", lambda e, fc=fc, sq=sq, n=n, gq=gq: e.tensor_tensor(out=hmid[gq][:, fc, 0:n], in0=ps[4 + (fc % 2)][:, 0:n],
                                                                               in1=sgt[sq][:, 0:n], op=ALU.mult),
                     r=[PK[4 + (fc % 2)], f"sgt{sq}"], w=[f"hmid{gq}"])
            for blk in range(nb):
                yq = ysi % 3
                ysi += 1
                for half in range(2):
                    bank = 6 + half
                    for fc in range(4):
                        S.op("pe", lambda e, fc=fc, half=half, bank=bank, gq=gq, blk=blk, q=q: e.matmul(
                            ps[bank][:, :], lhsT=hmid[gq][:, fc, blk * 128:(blk + 1) * 128], rhs=wd[q][:, fc, half * 512:(half + 1) * 512],
                            start=(fc == 0), stop=(fc == 3)), r=[f"hmid{gq}", f"wd{q}"], w=[PK[bank]])
                    if half == 0:
                        S.op("act", lambda e, yq=yq, bank=bank: e.copy(out=yst[yq][:, 0:512], in_=ps[bank][:, :]),
                             r=[PK[bank]], w=[f"yst{yq}a"])
                    else:
                        S.op("dve", lambda e, yq=yq, bank=bank: e.tensor_copy(out=yst[yq][:, 512:1024], in_=ps[bank][:, :]),
                             r=[PK[bank]], w=[f"yst{yq}b"])
                r0 = ex * CAP + goff + blk * 128
                S.dma("sp", lambda e, yq=yq, r0=r0: e.dma_start(out=yd_d[r0:r0 + 128, :], in_=yst[yq][:]),
                      r=[f"yst{yq}a", f"yst{yq}b"], w=["yd"], key=f"yst{yq}")
        if goff in SEC_END and DYN_SKIP:
            S.section_end()
    S.barrier()
    A.reset(PM)
    if stage < 2.9:
        return nc, S

    gfin = A("gfin", [128, D], F32)
    S.dma("sp", lambda e: e.dma_start(out=gfin[:], in_=vec_d[:, VOFF["g_final"][0]:VOFF["g_final"][0] + 1024]), w=["gfin"], key="gfin")
    x2s = [A("x2s", [128, D], F32) for _ in range(3)]
    yas = [A("ya", [128, D], F32) for _ in range(3)]
    ybs = [A("yb", [128, D], F32) for _ in range(3)]
    ots = [A("ot", [128, D], F32) for _ in range(3)]
    junkD = A("junkD", [128, D], BF16)
    ssD = [A("ssD", [128, 1], F32) for _ in range(2)]
    for q in range(3):
        S.op("pool", lambda e, q=q: e.memset(yas[q][:], 0.0), w=[f"ya{q}"])
        S.op("pool", lambda e, q=q: e.memset(ybs[q][:], 0.0), w=[f"yb{q}"])

    def load_d(i):
        q = i % 3
        S.dma("sp", lambda e: e.dma_start(out=x2s[q][:], in_=out_d[i * 128:(i + 1) * 128, :]), r=[f"out{i}"], w=[f"x2s{q}"], key=f"x2s{q}")
        for k, (buf, nm) in enumerate(((yas, "ya"), (ybs, "yb"))):
            if stage == 2.9 or (stage == 2.95 and i >= 8):
                continue
            S.dma("pool", lambda e, buf=buf, k=k: e.indirect_dma_start(
                out=buf[q][:, :], out_offset=None, in_=yd_d[:, :],
                in_offset=bass.IndirectOffsetOnAxis(ap=slots_i[:, i, k:k + 1], axis=0),
                bounds_check=_bcreg(e), oob_is_err=False), r=["yd", "slots"], w=[f"{nm}{q}", f"gath{k}"], key=f"{nm}{q}")

    load_d(0)
    load_d(1)
    for i in range(NT):
        q = i % 3
        if i + 2 < NT:
            load_d(i + 2)
        S.op("dve", lambda e, q=q, i=i: e.scalar_tensor_tensor(out=x2s[q][:], in0=yas[q][:], scalar=wts[:, i, 0:1], in1=x2s[q][:],
                                                               op0=ALU.mult, op1=ALU.add), r=[f"ya{q}", f"x2s{q}", "wts"], w=[f"x2s{q}"])
        S.op("dve", lambda e, q=q, i=i: e.scalar_tensor_tensor(out=x2s[q][:], in0=ybs[q][:], scalar=wts[:, i, 1:2], in1=x2s[q][:],
                                                                op0=ALU.mult, op1=ALU.add), r=[f"yb{q}", f"x2s{q}", "wts"], w=[f"x2s{q}"])
        rms_to_bf16(x2s[q][:], f"x2s{q}", gfin[:], "gfin", ots[q][:], f"ot{q}", junkD, ssD[0], ssD[1], "D")
        S.dma("sp", lambda e, q=q, i=i: e.dma_start(out=out_d[i * 128:(i + 1) * 128, :], in_=ots[q][:]),
              r=[f"ot{q}"], w=[f"out{i}"], key=f"ot{q}")
    S.barrier()
    return nc, S


_CACHE = {}


def _get_program(stage=3):
    if stage not in _CACHE:
        nc, S = build(stage)
        st = ExitStack()
        S.emit(st)
        _CACHE[stage] = (nc, st)
    return _CACHE[stage][0]


def _host_tables(inputs):
    f = lambda k: np.asarray(inputs[k], dtype=np.float32)
    rows = {
        "g_mix": f("g_mix")[0], "g_xattn": f("g_xattn")[0], "g_mem": f("g_mem")[0], "g_moe": f("g_moe")[0],
        "g_final": f("g_final"), "ln_g": f("ln_v_g")[0], "ln_b": f("ln_v_b")[0], "g_head": f("g_head")[0],
        "gate_b": f("gate_b")[0], "b_r": np.concatenate([f("b_rg")[0], f("b_re")[0]]),
        "iota": np.arange(32, dtype=np.float32),
    }
    vec = np.zeros((128, NVEC), np.float32)
    for k, (o, l) in VOFF.items():
        vec[:, o:o + l] = rows[k][None, :]
    col = np.zeros((128, NCOL), np.float32)
    cw = f("conv_w")[0][:, 0, :]
    col[:, 0:40] = cw.reshape(5, 8, 128).transpose(2, 1, 0).reshape(128, 40)
    col[:, 40:48] = f("conv_b")[0].reshape(8, 128).T
    col[:, 48:52] = f("b_s")[0].T
    r = np.arange(128)
    cst = np.zeros((128, 640), np.float32)
    cst[:, 0:128] = np.eye(128)
    cst[:, 128:256] = (r[:, None] <= r[None, :])
    cst[:, 256:384] = (r[:, None] >= r[None, :])
    cst[:, 384:512] = 1.0
    cst[:, 512:640] = (r[:, None] < r[None, :])
    w_sT = np.ascontiguousarray(f("w_s")[0].transpose(2, 0, 1))
    w_r = np.ascontiguousarray(np.concatenate([f("w_rg")[0], f("w_re")[0]], axis=1))
    return vec, col, cst, w_sT, w_r


def make_in_maps(inputs):
    vec, col, cst, w_sT, w_r = _host_tables(inputs)
    f = lambda k: np.ascontiguousarray(np.asarray(inputs[k], dtype=np.float32)[0])
    shared = {
        "w_in": f("w_in"), "w_out": f("w_out"), "w_q": f("w_q_x"), "w_kv": f("w_kv_x"), "w_o": f("w_o_x"),
        "w_r": w_r, "w_gate": f("w_gate"), "w_up": f("w_up"), "w_down": f("w_down"),
        "w_sT": w_sT, "vec": vec, "col": col, "cst": cst,
    }
    x = np.asarray(inputs["x"], dtype=np.float32)
    mem = np.asarray(inputs["mem"], dtype=np.float32)
    maps = []
    for c in range(NCORES):
        m = dict(shared)
        m["x"] = np.ascontiguousarray(x[2 * c:2 * c + 2].reshape(NTOK, D))
        m["mem"] = np.ascontiguousarray(mem[2 * c:2 * c + 2].reshape(512, D))
        maps.append(m)
    return maps


def kernel(**inputs):
    nc = _get_program(3)
    maps = make_in_maps(inputs)
    res = run_bass_kernel_spmd(nc, maps, core_ids=list(range(NCORES)))
    out = np.concatenate([np.asarray(r["out"]).reshape(2, SEQ, D) for r in res.results], axis=0)
    return out.astype(np.float32)
```

```python
import numpy as np
from contextlib import ExitStack
import concourse.bass as bass
import concourse.mybir as mybir
from concourse.bass_utils import run_bass_kernel_spmd

F32 = mybir.dt.float32
BF16 = mybir.dt.bfloat16
I32 = mybir.dt.int32
U32 = mybir.dt.uint32
ALU = mybir.AluOpType
AF = mybir.ActivationFunctionType
AX = mybir.AxisListType

ENGS = ("pe", "act", "dve", "pool", "sp")
EPOCH = 8000
DMA_EPOCH = 30000

NCORES = 8
D = 1024
NTOK = 8192
NT = NTOK // 128
SEQ = 4096
NCH = 32
PROJ = 3088
CAP = 1280
NSLOT = 32 * CAP
SBUF_BASE = 16512
SBUF_END = 229376
SERIAL = False
DYN_SKIP = True


class _Op:
    __slots__ = ("fn", "deps", "signal", "dma_key", "dma_val", "sigval", "sec")

    def __init__(self, fn, deps, dma_key=None, dma_val=None):
        self.fn = fn
        self.deps = deps
        self.signal = False
        self.dma_key = dma_key
        self.dma_val = dma_val
        self.sigval = None
        self.sec = None


class Sched:
    def __init__(self, nc):
        self.nc = nc
        self.ops = {e: [] for e in ENGS}
        self.last_w = {}
        self.readers = {}
        self.dma_cnt = {}
        self.secs = []
        self.cur_sec = None

    def section_begin(self, cnt_ap, thr, tag=None):
        self.secs.append((cnt_ap, thr, tag))
        self.cur_sec = len(self.secs) - 1

    def section_end(self):
        self.cur_sec = None

    def _deps(self, eng, r, w, is_dma):
        deps = []
        for k in r:
            t = self.last_w.get(k)
            if t is not None:
                deps.append((t, "raw"))
        for k in w:
            t = self.last_w.get(k)
            if t is not None:
                deps.append((t, "waw"))
            for t in self.readers.get(k, ()):
                deps.append((t, "war"))
        out = []
        seen = set()
        for t, kind in deps:
            if t in seen:
                continue
            if t[0] == "c" and t[1] == eng and not is_dma:
                if eng == "pe" or kind == "war":
                    continue
            seen.add(t)
            out.append(t)
        best = {}
        res = []
        for t in out:
            if t[0] == "c":
                if t[1] not in best or best[t[1]][2] < t[2]:
                    best[t[1]] = t
            else:
                res.append(t)
        return res + list(best.values())

    def _commit(self, tok, r, w):
        for k in w:
            self.last_w[k] = tok
            self.readers[k] = []
        for k in r:
            self.readers.setdefault(k, []).append(tok)

    serial = False

    def _all_toks(self, eng):
        toks = []
        for e in ENGS:
            if e == eng and e == "pe":
                continue
            n = len(self.ops[e])
            for i in range(n - 1, -1, -1):
                if self.ops[e][i].dma_key is None and self.ops[e][i].fn is not None:
                    toks.append(("c", e, i))
                    break
        for key, (gen, cnt) in self.dma_cnt.items():
            toks.append(("d", key, gen, cnt))
        return toks

    def op(self, eng, fn, r=(), w=()):
        deps = self._deps(eng, r, w, False)
        if self.serial is True or self.serial == eng:
            deps = self._all_toks(eng)
        idx = len(self.ops[eng])
        o = _Op(fn, deps)
        o.sec = self.cur_sec
        self.ops[eng].append(o)
        self._commit(("c", eng, idx), r, w)

    def dma(self, eng, fn, r=(), w=(), key=None):
        assert key is not None
        deps = self._deps(eng, r, w, True)
        if self.serial is True or self.serial == "dma":
            deps = self._all_toks(None)
        gen, cnt = self.dma_cnt.get(key, (0, 0))
        if cnt + 16 > DMA_EPOCH:
            gen, cnt = gen + 1, 0
        cnt += 16
        self.dma_cnt[key] = (gen, cnt)
        o = _Op(fn, deps, dma_key=(key, gen), dma_val=cnt)
        o.sec = self.cur_sec
        self.ops[eng].append(o)
        self._commit(("d", key, gen, cnt), r, w)

    def barrier(self):
        toks = []
        for e in ENGS:
            n = len(self.ops[e])
            for i in range(n - 1, -1, -1):
                if self.ops[e][i].dma_key is None and self.ops[e][i].fn is not None:
                    toks.append(("c", e, i))
                    break
        for key, (gen, cnt) in self.dma_cnt.items():
            toks.append(("d", key, gen, cnt))
        for e in ENGS:
            deps = [t for t in toks if not (t[0] == "c" and t[1] == e)]
            self.ops[e].append(_Op(None, deps))
        self.last_w = {}
        self.readers = {}

    def emit(self, stack):
        nc = self.nc
        for e in ENGS:
            for o in self.ops[e]:
                for t in o.deps:
                    if t[0] == "c":
                        self.ops[t[1]][t[2]].signal = True
        csem = {}
        for e in ENGS:
            cnt = 0
            for o in self.ops[e]:
                if o.signal:
                    cnt += 1
                    o.sigval = cnt
            nep = max((cnt + EPOCH - 1) // EPOCH, 1)
            csem[e] = [stack.enter_context(nc.semaphore(f"c_{e}_{i}")) for i in range(nep)]
        dsem = {}
        for e in ENGS:
            for o in self.ops[e]:
                if o.dma_key is not None and o.dma_key not in dsem:
                    dsem[o.dma_key] = stack.enter_context(nc.semaphore(f"d{len(dsem)}"))
        self.n_sems = sum(len(v) for v in csem.values()) + len(dsem)

        def sem_of(e, sigval):
            ep = (sigval - 1) // EPOCH
            return csem[e][ep], sigval - ep * EPOCH

        block = stack.enter_context(nc.Block())
        ops = self.ops

        secs = self.secs

        def run(e, engobj):
            waited_c = {x: 0 for x in ENGS}
            waited_d = {}
            creg = [None]

            def emit_one(o):
                for t in o.deps:
                    if t[0] == "c":
                        sv = ops[t[1]][t[2]].sigval
                        if sv <= waited_c[t[1]]:
                            continue
                        waited_c[t[1]] = sv
                        s_, v_ = sem_of(t[1], sv)
                        engobj.wait_ge(s_, v_)
                    else:
                        k = (t[1], t[2])
                        if waited_d.get(k, 0) >= t[3]:
                            continue
                        waited_d[k] = t[3]
                        engobj.wait_ge(dsem[k], t[3])
                if o.fn is None:
                    return
                ins = o.fn(engobj)
                if o.dma_key is not None:
                    ins.then_inc(dsem[o.dma_key], 16)
                elif o.signal:
                    s_, v_ = sem_of(e, o.sigval)
                    ins.then_inc(s_, 1)

            lst = ops[e]
            i = 0
            while i < len(lst):
                o = lst[i]
                if o.sec is None:
                    emit_one(o)
                    i += 1
                    continue
                j = i
                while j < len(lst) and lst[j].sec == o.sec:
                    j += 1
                group = lst[i:j]
                i = j
                cnt_ap, thr, tag = secs[o.sec]
                if creg[0] is None:
                    creg[0] = engobj.alloc_register("secreg")
                    creg.append(object())
                if tag is None or creg[1] != tag:
                    engobj.reg_load(creg[0], cnt_ap)
                    creg[1] = tag
                ccomp = {}
                dcomp = {}
                for g in group:
                    if g.dma_key is not None:
                        ent = dcomp.setdefault(g.dma_key, [dsem[g.dma_key], 0, g.dma_val - 16])
                        ent[1] += 16
                    elif g.signal:
                        s_, v_ = sem_of(e, g.sigval)
                        ent = ccomp.setdefault(id(s_), [s_, 0, v_ - 1])
                        ent[1] += 1
                saved_c = dict(waited_c)
                saved_d = dict(waited_d)
                with engobj.If_lt(creg[0], thr + 1):
                    for (s_, n_, pre_) in list(ccomp.values()) + list(dcomp.values()):
                        if pre_ > 0:
                            engobj.wait_ge(s_, pre_)
                        engobj.sem_inc(s_, n_)
                with engobj.Else():
                    for g in group:
                        emit_one(g)
                waited_c.clear()
                waited_c.update(saved_c)
                waited_d.clear()
                waited_d.update(saved_d)

        @block.tensor
        def _(eng):
            run("pe", eng)

        @block.scalar
        def _(eng):
            run("act", eng)

        @block.vector
        def _(eng):
            run("dve", eng)

        @block.gpsimd
        def _(eng):
            run("pool", eng)

        @block.sync
        def _(eng):
            run("sp", eng)


_BC = {}


def _bcreg(e):
    k = id(e)
    if k not in _BC:
        r = e.alloc_register("bcreg")
        e.reg_mov(r, NSLOT - 1)
        _BC[k] = (e, r)
    return _BC[k][1]


class Alloc:
    def __init__(self, nc):
        self.nc = nc
        self.off = SBUF_BASE
        self.n = 0

    def mark(self):
        return self.off

    def reset(self, m):
        self.off = m

    def __call__(self, name, shape, dt):
        esz = 2 if dt == BF16 else 4
        nb = int(np.prod(shape[1:])) * esz
        nb = (nb + 63) // 64 * 64
        assert self.off + nb <= SBUF_END, (name, self.off, nb)
        self.n += 1
        t = self.nc.alloc_sbuf_tensor_at(f"{name}_{self.n}", list(shape), dt, offset=self.off)
        self.off += nb
        return t


VOFF = {}
_o = 0
for _n, _l in [("g_mix", 1024), ("g_xattn", 1024), ("g_mem", 1024), ("g_moe", 1024), ("g_final", 1024),
               ("ln_g", 512), ("ln_b", 512), ("g_head", 512), ("gate_b", 16), ("b_r", 36), ("iota", 32)]:
    VOFF[_n] = (_o, _l)
    _o += _l
NVEC = _o
NCOL = 8 * 5 + 8 + 4


def build(stage=3, debug=False):
    nc = bass.Bass("TRN2", target_bir_lowering=False)

    def din(name, shape, dt=F32):
        return nc.dram_tensor(name, list(shape), dt, kind="ExternalInput").ap()

    x_d = din("x", [NTOK, D])
    mem_d = din("mem", [512, D])
    w_in_d = din("w_in", [D, PROJ])
    w_out_d = din("w_out", [D, D])
    w_q_d = din("w_q", [D, D])
    w_kv_d = din("w_kv", [D, 2 * D])
    w_o_d = din("w_o", [D, D])
    w_r_d = din("w_r", [D, 36])
    w_gate_d = din("w_gate", [32, D, 512])
    w_up_d = din("w_up", [32, D, 512])
    w_down_d = din("w_down", [32, 512, D])
    w_sT_d = din("w_sT", [128, 4, 128])
    vec_d = din("vec", [128, NVEC])
    col_d = din("col", [128, NCOL])
    cst_d = din("cst", [128, 5 * 128])
    out_d = nc.dram_tensor("out", [NTOK, D], F32, kind="ExternalOutput").ap()

    SK = "ExternalOutput" if debug else "Internal"
    zqk_d = nc.dram_tensor("zqk_s", [D, NTOK], BF16, kind=SK).ap()
    v_d = nc.dram_tensor("v_s", [NTOK, 512], BF16, kind=SK).ap()
    og_d = nc.dram_tensor("og_s", [NTOK, 512], F32, kind=SK).ap()
    y_d = nc.dram_tensor("y_s", [NTOK, D], BF16, kind=SK).ap()
    xd_d = nc.dram_tensor("xd_s", [NSLOT, D], BF16, kind="Internal").ap()
    yd_d = nc.dram_tensor("yd_s", [NSLOT, D], F32, kind="Internal").ap()

    S = Sched(nc)
    S.serial = SERIAL
    A = Alloc(nc)
    ps = [nc.alloc_psum_tensor(f"ps{i}", [128, 512], F32) for i in range(8)]
    PK = [f"ps{i}" for i in range(8)]

    cst = A("cst", [128, 640], F32)
    ident_b = A("identb", [128, 128], BF16)
    ones_b = A("onesb", [128, 128], BF16)
    strict_b = A("strictb", [128, 128], BF16)
    col = A("col", [128, NCOL], F32)
    gates_all = A("gates", [128, NT, 16], F32)
    slots_i = A("slots", [128, NT, 2], I32)
    wts = A("wts", [128, NT, 2], F32)
    cnt_i = A("cnt_i", [128, 32], I32)
    ident_f = cst[:, 0:128]
    maskf = cst[:, 128:256]
    maskb = cst[:, 256:384]
    ones_f = cst[:, 384:512]
    strict = cst[:, 512:640]

    S.dma("sp", lambda e: e.dma_start(out=cst[:], in_=cst_d), w=["cst"], key="cst")
    S.dma("sp", lambda e: e.dma_start(out=col[:], in_=col_d), w=["col"], key="col")
    S.op("dve", lambda e: e.tensor_copy(out=ident_b[:], in_=cst[:, 0:128]), r=["cst"], w=["identb"])
    S.op("dve", lambda e: e.tensor_copy(out=ones_b[:], in_=cst[:, 384:512]), r=["cst"], w=["onesb"])
    S.op("dve", lambda e: e.tensor_copy(out=strict_b[:], in_=cst[:, 512:640]), r=["cst"], w=["strictb"])

    PM = A.mark()

    def load_w(dst, src_d, key, nk, eng="pool"):
        for kc in range(nk):
            S.dma(eng, lambda e, kc=kc: e.dma_start(out=dst[:, kc, :], in_=src_d[kc * 128:(kc + 1) * 128, :]),
                  w=[key], key=key)

    _cast_rr = [0]

    def load_w_fast(dst, src_d, key, nk, stgs, queue="sp"):
        n_ = dst.shape[2]
        for kc in range(nk):
            si = _cast_rr[0] % len(stgs)
            stg_ap, skey = stgs[si]
            eng = ("dve", "act", "pool")[_cast_rr[0] % 3]
            _cast_rr[0] += 1
            S.dma(queue, lambda e, kc=kc, stg_ap=stg_ap: e.dma_start(out=stg_ap[:, 0:n_], in_=src_d[kc * 128:(kc + 1) * 128, :]),
                  w=[skey], key=skey)
            if eng == "act":
                S.op("act", lambda e, kc=kc, stg_ap=stg_ap: e.copy(out=dst[:, kc, :], in_=stg_ap[:, 0:n_]), r=[skey], w=[key])
            else:
                S.op(eng, lambda e, kc=kc, stg_ap=stg_ap: e.tensor_copy(out=dst[:, kc, :], in_=stg_ap[:, 0:n_]), r=[skey], w=[key])

    def rms_to_bf16(xt_ap, xkey, g_ap, gkey, out_ap, okey, junk, ss, rstd, tag):
        S.op("act", lambda e: e.activation(out=junk[:], in_=xt_ap, func=AF.Square, accum_out=ss[:]),
             r=[xkey], w=["junk" + tag, "ss" + tag])
        S.op("act", lambda e: e.activation(out=rstd[:], in_=ss[:], func=AF.Sqrt, scale=1.0 / D, bias=1e-6),
             r=["ss" + tag], w=["rstd" + tag])
        S.op("dve", lambda e: e.reciprocal(out=rstd[:], in_=rstd[:]), r=["rstd" + tag], w=["rstd" + tag])
        S.op("dve", lambda e: e.scalar_tensor_tensor(out=out_ap, in0=xt_ap, scalar=rstd[:, 0:1], in1=g_ap,
                                                     op0=ALU.mult, op1=ALU.mult),
             r=[xkey, "rstd" + tag, gkey], w=[okey])

    def transpose8(src, skey, dst_ap, dkey, pbank, evac="act"):
        pb = ps[pbank][:].bitcast(BF16)
        for c in range(8):
            S.op("pe", lambda e, c=c: e.transpose(out=pb[:, c * 128:(c + 1) * 128], in_=src[:, c * 128:(c + 1) * 128],
                                                  identity=ident_b[:]),
                 r=[skey, "identb"], w=[PK[pbank]])
        src_v = pb[:, 0:1024].rearrange("p (c n) -> p c n", c=8)
        if evac == "act":
            S.op("act", lambda e: e.copy(out=dst_ap, in_=src_v), r=[PK[pbank]], w=[dkey])
        else:
            S.op("dve", lambda e: e.tensor_copy(out=dst_ap, in_=src_v), r=[PK[pbank]], w=[dkey])

    w_in_b = A("w_in", [128, 8, PROJ], BF16)
    w_sT_f = A("w_sTf", [128, 4, 128], F32)
    w_sT_b = A("w_sTb", [128, 4, 128], BF16)
    NVA = 1024 + 512 + 512 + 16
    vecA = A("vecA", [128, NVA], F32)
    gmix = vecA[:, 0:1024]
    ln_g = vecA[:, 1024:1536]
    ln_b = vecA[:, 1536:2048]
    gate_b = vecA[:, 2048:2064]
    S.dma("sp", lambda e: e.dma_start(out=vecA[:, 0:1024], in_=vec_d[:, VOFF["g_mix"][0]:VOFF["g_mix"][0] + 1024]), w=["vecA"], key="vecA")
    S.dma("sp", lambda e: e.dma_start(out=vecA[:, 1024:2048], in_=vec_d[:, VOFF["ln_g"][0]:VOFF["ln_g"][0] + 1024]), w=["vecA"], key="vecA")
    S.dma("sp", lambda e: e.dma_start(out=vecA[:, 2048:2064], in_=vec_d[:, VOFF["gate_b"][0]:VOFF["gate_b"][0] + 16]), w=["vecA"], key="vecA")
    S.dma("sp", lambda e: e.dma_start(out=w_sT_f[:], in_=w_sT_d), w=["w_sTf"], key="w_sTf")
    S.op("dve", lambda e: e.tensor_copy(out=w_sT_b[:], in_=w_sT_f[:]), r=["w_sTf"], w=["w_sTb"])
    stgA = [A("stgA", [128, PROJ], F32) for _ in range(4)]
    load_w_fast(w_in_b, w_in_d, "w_in", 8, [(stgA[q_], f"stgA{q_}") for q_ in range(4)])
    zt = A("zt", [128, 8192], BF16)
    S.op("pool", lambda e: e.memset(zt[:], 0.0), w=["zt"])
    rows_per = 128 * 8
    zf_todo = list(range(NSLOT // rows_per))

    def zfill_one():
        if zf_todo:
            k = zf_todo.pop(0)
            S.dma("pool", lambda e, k=k: e.dma_start(
                out=xd_d[k * rows_per:(k + 1) * rows_per, :].rearrange("(p r) n -> p (r n)", p=128), in_=zt[:]),
                r=["zt"], w=["xd"], key="zfill")


    xts = [A("xt", [128, D], F32) for _ in range(3)]
    junk = A("junk", [128, D], BF16)
    ssA = A("ss", [128, 1], F32)
    rstdA = A("rstd", [128, 1], F32)
    xns = [A("xn", [128, D], BF16) for _ in range(2)]
    xnTs = [A("xnT", [128, 8, 512], BF16) for _ in range(2)]
    zst = [A("zst", [128, 512], BF16) for _ in range(2)]
    vst = [A("vst", [128, 512], BF16) for _ in range(2)]
    ogst = [A("ogst", [128, 512], F32) for _ in range(2)]
    gus = [A("gu", [128, 512], F32) for _ in range(2)]
    gvs = [A("gv", [128, 512], F32) for _ in range(2)]
    gvt = A("gvt", [128, 512], F32)
    gvn = [A("gvn", [128, 512], BF16) for _ in range(2)]
    ygs = [A("yg", [128, 512], BF16) for _ in range(2)]
    st5 = A("st5", [128, 8], F32)

    def load_x(i):
        S.dma("sp", lambda e: e.dma_start(out=xts[i % 3][:], in_=x_d[i * 128:(i + 1) * 128, :]),
              w=[f"xt{i % 3}"], key=f"xt{i % 3}")

    def front(i):
        seg_, j_ = divmod(i, 4)
        p_ = i % 3
        if i + 2 < NT:
            load_x(i + 2)
        rms_to_bf16(xts[p_][:], f"xt{p_}", gmix, "vecA", xns[i % 2][:], f"xn{i % 2}", junk, ssA, rstdA, "A")
        transpose8(xns[i % 2], f"xn{i % 2}", xnTs[seg_ % 2][:, :, j_ * 128:(j_ + 1) * 128], f"xnT{seg_ % 2}_{j_}", 0, evac="dve")

    load_x(0)
    load_x(1)
    front(0)
    front(1)
    pending = []
    for seg in range(NT // 4):
        sb = seg % 2
        xnT = xnTs[sb]
        for j in range(4):
            i = seg * 4 + j
            p = i % 2
            blocks = [(2, 1024, 512), (3, 1536, 512), (6, 2048, 16), (4, 2064, 512), (5, 2576, 512)]
            for (bank, c0, n) in blocks:
                for kc in range(8):
                    S.op("pe", lambda e, bank=bank, c0=c0, n=n, kc=kc, j=j, xnT=xnT: e.matmul(
                        ps[bank][:, 0:n], lhsT=xnT[:, kc, j * 128:(j + 1) * 128], rhs=w_in_b[:, kc, c0:c0 + n],
                        start=(kc == 0), stop=(kc == 7)),
                        r=[f"xnT{sb}_{j}", "w_in"], w=[PK[bank]])
            if i + 2 < NT:
                front(i + 2)
            S.op("dve", lambda e, p=p: e.tensor_copy(out=vst[p][:], in_=ps[2][:, :]), r=[PK[2]], w=[f"vst{p}"])
            S.dma("pool", lambda e, p=p, i=i: e.dma_start(out=v_d[i * 128:(i + 1) * 128, :], in_=vst[p][:]),
                  r=[f"vst{p}"], w=["v_d"], key=f"vst{p}")
            zfill_one()
            S.op("act", lambda e, p=p: e.activation(out=ogst[p][:], in_=ps[3][:, :], func=AF.Sigmoid),
                 r=[PK[3]], w=[f"ogst{p}"])
            S.dma("pool", lambda e, p=p, i=i: e.dma_start(out=og_d[i * 128:(i + 1) * 128, :], in_=ogst[p][:]),
                  r=[f"ogst{p}"], w=["og_d"], key=f"ogst{p}")
            S.op("dve", lambda e, i=i: e.tensor_tensor(out=gates_all[:, i, :], in0=ps[6][:, 0:16], in1=gate_b, op=ALU.add),
                 r=[PK[6], "vecA"], w=["gates"])
            S.op("act", lambda e, p=p: e.activation(out=gus[p][:], in_=ps[4][:, :], func=AF.Gelu_apprx_tanh),
                 r=[PK[4]], w=[f"gu{p}"])
            S.op("act", lambda e, p=p: e.activation(out=gvs[p][:], in_=ps[5][:, :], func=AF.Gelu_apprx_tanh,
                                                    accum_out=st5[:, 0:1]),
                 r=[PK[5]], w=[f"gv{p}", "st_s1"])
            S.op("act", lambda e, p=p: e.activation(out=junk[:, 0:512], in_=gvs[p][:], func=AF.Square,
                                                    accum_out=st5[:, 1:2]),
                 r=[f"gv{p}"], w=["junkA", "st_s2"])
            S.op("dve", lambda e: e.tensor_scalar(out=st5[:, 2:3], in0=st5[:, 0:1], scalar1=1.0 / 512, scalar2=None,
                                                  op0=ALU.mult), r=["st_s1"], w=["st_m"])
            S.op("dve", lambda e: e.tensor_tensor(out=st5[:, 3:4], in0=st5[:, 2:3], in1=st5[:, 2:3], op=ALU.mult),
                 r=["st_m"], w=["st_msq"])
            S.op("dve", lambda e: e.scalar_tensor_tensor(out=st5[:, 4:5], in0=st5[:, 1:2], scalar=1.0 / 512,
                                                         in1=st5[:, 3:4], op0=ALU.mult, op1=ALU.subtract),
                 r=["st_s2", "st_msq"], w=["st_var"])
            S.op("act", lambda e: e.activation(out=st5[:, 5:6], in_=st5[:, 4:5], func=AF.Sqrt, bias=1e-5),
                 r=["st_var"], w=["st_sd"])
            S.op("dve", lambda e: e.reciprocal(out=st5[:, 6:7], in_=st5[:, 5:6]), r=["st_sd"], w=["st_rs"])
            S.op("dve", lambda e, p=p: e.tensor_scalar(out=gvt[:], in0=gvs[p][:], scalar1=st5[:, 2:3], scalar2=st5[:, 6:7],
                                                       op0=ALU.subtract, op1=ALU.mult),
                 r=[f"gv{p}", "st_m", "st_rs"], w=["gvt"])
            S.op("pool", lambda e: e.tensor_tensor(out=gvt[:], in0=gvt[:], in1=ln_g, op=ALU.mult),
                 r=["gvt", "vecA"], w=["gvt"])
            S.op("pool", lambda e, p=p: e.tensor_tensor(out=gvn[p][:], in0=gvt[:], in1=ln_b, op=ALU.add),
                 r=["gvt", "vecA"], w=[f"gvn{p}"])
            def back2(p=p, i=i):
                for g in range(4):
                    S.op("pe", lambda e, g=g, p=p: e.matmul(ps[7][:, g * 128:(g + 1) * 128], lhsT=w_sT_b[:, g, :],
                                                            rhs=gvn[p][:, g * 128:(g + 1) * 128], start=True, stop=True),
                         r=["w_sTb", f"gvn{p}"], w=[PK[7]])
                for g in range(4):
                    S.op("dve", lambda e, g=g, p=p: e.scalar_tensor_tensor(
                        out=ygs[p][:, g * 128:(g + 1) * 128], in0=ps[7][:, g * 128:(g + 1) * 128],
                        scalar=col[:, 48 + g:49 + g], in1=gus[p][:, g * 128:(g + 1) * 128], op0=ALU.add, op1=ALU.mult),
                        r=[PK[7], "col", f"gu{p}"], w=[f"yg{p}"])
                S.dma("pool", lambda e, p=p, i=i: e.dma_start(out=y_d[i * 128:(i + 1) * 128, 512:1024], in_=ygs[p][:]),
                      r=[f"yg{p}"], w=["y_d"], key=f"yg{p}")
            if pending:
                pending.pop()()
            pending.append(back2)
        for fc in range(8):
            zp = fc % 2
            zb = 1 if fc % 2 == 0 else 6
            for kc in range(8):
                S.op("pe", lambda e, fc=fc, kc=kc, xnT=xnT, zb=zb: e.matmul(
                    ps[zb][:, :], lhsT=w_in_b[:, kc, fc * 128:(fc + 1) * 128], rhs=xnT[:, kc, :],
                    start=(kc == 0), stop=(kc == 7)), r=[f"xnT{sb}_{jx}" for jx in range(4)] + ["w_in"], w=[PK[zb]])
            S.op("dve", lambda e, zp=zp, zb=zb: e.tensor_copy(out=zst[zp][:], in_=ps[zb][:, :]), r=[PK[zb]], w=[f"zst{zp}"])
            S.dma("pool", lambda e, zp=zp, fc=fc, seg=seg: e.dma_start(
                out=zqk_d[fc * 128:(fc + 1) * 128, seg * 512:(seg + 1) * 512], in_=zst[zp][:]),
                r=[f"zst{zp}"], w=["zqk_d"], key=f"zst{zp}")
    while pending:
        pending.pop()()
    while zf_todo:
        zfill_one()
    S.barrier()
    A.reset(PM)
    if stage == 0:
        return nc, S

    ghead = A("ghead", [128, 512], F32)
    S.dma("sp", lambda e: e.dma_start(out=ghead[:], in_=vec_d[:, VOFF["g_head"][0]:VOFF["g_head"][0] + 512]), w=["ghead"], key="ghead")
    zcs = [A("zc", [128, SEQ + 4], BF16) for _ in range(2)]
    diag = A("diag", [128, 8, 5, 128], BF16)
    og_t = A("og_t", [128, NCH, 128], F32)
    hs = A("hs", [128, NCH, 128], F32)
    ybf = A("ybf", [128, NCH, 128], BF16)
    qT = A("qT", [128, SEQ], BF16)
    kT = A("kT", [128, SEQ], BF16)
    vaug = A("vaug", [128, NCH, 129], BF16)
    ktok = A("ktok", [128, NCH, 128], BF16)
    Vp = [A("Vp", [128, NCH, 129], BF16) for _ in range(2)]
    PTa = [A("PTa", [128, NCH, 128], BF16) for _ in range(2)]
    Z = [A("Z", [128, NCH, 129], F32) for _ in range(2)]
    Zb = [A("Zb", [128, NCH + 1, 129], BF16) for _ in range(2)]
    gsm = A("gsm", [128, 16, NCH], F32)
    S.op("pool", lambda e: e.memset(zcs[0][:], 0.0), w=["zc0"])
    S.op("pool", lambda e: e.memset(zcs[1][:], 0.0), w=["zc1"])
    S.op("pool", lambda e: e.memset(vaug[:], 1.0), w=["vaug"])
    for fc_ in range(8):
        for jj_ in range(5):
            S.op("dve", lambda e, fc_=fc_, jj_=jj_: e.tensor_scalar(out=diag[:, fc_, jj_, :], in0=cst[:, 0:128],
                                                                 scalar1=col[:, fc_ * 5 + jj_:fc_ * 5 + jj_ + 1], scalar2=None,
                                                                 op0=ALU.mult), r=["cst", "col"], w=["diag"])
    S.op("pool", lambda e: e.memset(Zb[0][:], 0.0), w=["Zb0"])
    S.op("pool", lambda e: e.memset(Zb[1][:], 0.0), w=["Zb1"])
    if stage == -1:
        S.barrier()
        return nc, S
    masks = [maskf, maskb]
    SCALE = 128 ** -0.5
    import math
    LNS = math.log(SCALE)
    first_bh = True

    for b in range(2):
        for h in range(4):
            for wi, (fc, dst, dkey) in enumerate([(h, qT, "qT"), (4 + h, kT, "kT")]):
                zc = zcs[wi]
                zk = f"zc{wi}"
                S.dma("sp", lambda e, zc=zc, fc=fc, b=b: e.dma_start(
                    out=zc[:, 2:2 + SEQ], in_=zqk_d[fc * 128:(fc + 1) * 128, b * SEQ:(b + 1) * SEQ]),
                    w=[zk], key=zk)
                for cb in range(8):
                    bank = cb % 2
                    for jj in range(5):
                        S.op("pe", lambda e, zc=zc, fc=fc, cb=cb, jj=jj, bank=bank: e.matmul(
                            ps[bank][:, :], lhsT=diag[:, fc, jj, :], rhs=zc[:, cb * 512 + jj:cb * 512 + jj + 512],
                            start=(jj == 0), stop=(jj == 4)), r=["diag", zk], w=[PK[bank]])
                    S.op("act", lambda e, dst=dst, cb=cb, fc=fc, bank=bank: e.activation(
                        out=dst[:, cb * 512:(cb + 1) * 512], in_=ps[bank][:, :], func=AF.Silu, bias=col[:, 40 + fc:41 + fc]),
                        r=[PK[bank], "col"], w=[dkey])
            first_bh = False
            S.dma("sp", lambda e, b=b, h=h: e.dma_start(
                out=vaug[:, :, 0:128], in_=v_d[b * SEQ:(b + 1) * SEQ, h * 128:(h + 1) * 128].rearrange("(c p) d -> p c d", p=128)),
                w=["vaug"], key="vaug")
            S.dma("sp", lambda e, b=b, h=h: e.dma_start(
                out=og_t[:], in_=og_d[b * SEQ:(b + 1) * SEQ, h * 128:(h + 1) * 128].rearrange("(c p) d -> p c d", p=128)),
                w=["og_t"], key="og_t")
            pb6 = ps[6][:].bitcast(BF16)
            for g8 in range(4):
                for cc in range(8):
                    c = g8 * 8 + cc
                    S.op("pe", lambda e, c=c, cc=cc: e.transpose(out=pb6[:, cc * 128:(cc + 1) * 128],
                                                                 in_=kT[:, c * 128:(c + 1) * 128], identity=ident_b[:]),
                         r=["kT", "identb"], w=[PK[6]])
                S.op("act", lambda e, g8=g8: e.copy(out=ktok[:, g8 * 8:(g8 + 1) * 8, :],
                                                    in_=pb6[:, 0:1024].rearrange("p (c n) -> p c n", c=8)),
                     r=[PK[6]], w=["ktok"])
            for d in range(2):
                icol = (0 if d == 0 else 8) + h
                fcol = (4 if d == 0 else 12) + h
                gi = gates_all[:, b * NCH:(b + 1) * NCH, icol]
                gf = gates_all[:, b * NCH:(b + 1) * NCH, fcol]
                S.op("act", lambda e, gf=gf: e.activation(out=gsm[:, 10, :], in_=gf, func=AF.Exp, scale=-1.0),
                     r=["gates"], w=["g_tmp"])
                S.op("act", lambda e, d=d: e.activation(out=gsm[:, d, :], in_=gsm[:, 10, :], func=AF.Ln, bias=1.0),
                     r=["g_tmp"], w=[f"g_spl{d}"])
                S.op("pe", lambda e, d=d: e.matmul(ps[7][:, d * 64:d * 64 + 32], lhsT=masks[d], rhs=gsm[:, d, :],
                                                   start=True, stop=True), r=["cst", f"g_spl{d}"], w=[PK[7]])
                S.op("pe", lambda e, d=d: e.matmul(ps[7][:, d * 64 + 32:d * 64 + 64], lhsT=ones_f, rhs=gsm[:, d, :],
                                                   start=True, stop=True), r=["cst", f"g_spl{d}"], w=[PK[7]])
                S.op("act", lambda e, d=d: e.activation(out=gsm[:, 2 + d, :], in_=ps[7][:, d * 64:d * 64 + 32], func=AF.Exp,
                                                        scale=-1.0, bias=LNS), r=[PK[7]], w=[f"g_rs{d}"])
                S.op("dve", lambda e, d=d, gi=gi: e.tensor_tensor(out=gsm[:, 10, :], in0=ps[7][:, d * 64:d * 64 + 32], in1=gi,
                                                                  op=ALU.add), r=[PK[7], "gates"], w=["g_tmp"])
                S.op("act", lambda e, d=d: e.activation(out=gsm[:, 4 + d, :], in_=gsm[:, 10, :], func=AF.Exp),
                     r=["g_tmp"], w=[f"g_u{d}"])
                S.op("act", lambda e, d=d: e.activation(out=gsm[:, 8 + d, :], in_=ps[7][:, d * 64 + 32:d * 64 + 64],
                                                        func=AF.Exp, scale=-1.0), r=[PK[7]], w=[f"g_eF{d}"])
                S.op("dve" if d == 0 else "pool", lambda e, d=d: e.tensor_tensor(out=Vp[d][:], in0=vaug[:],
                                                            in1=gsm[:, 4 + d, :].unsqueeze(2).to_broadcast([128, NCH, 129]),
                                                            op=ALU.mult), r=["vaug", f"g_u{d}"], w=[f"Vp{d}"])
            for g4 in range(NCH // 4):
                bank = g4 % 2
                for cc in range(4):
                    c = g4 * 4 + cc
                    cs = slice(c * 128, (c + 1) * 128)
                    S.op("pe", lambda e, cs=cs, cc=cc, bank=bank: e.matmul(ps[bank][:, cc * 128:(cc + 1) * 128], lhsT=kT[:, cs],
                                                                           rhs=qT[:, cs], start=True, stop=True),
                         r=["kT", "qT"], w=[PK[bank]])
                for d in range(2):
                    S.op("dve", lambda e, d=d, g4=g4, bank=bank: e.tensor_tensor(
                        out=PTa[d][:, g4 * 4:(g4 + 1) * 4, :], in0=ps[bank][:, :].rearrange("p (c n) -> p c n", c=4),
                        in1=masks[d].unsqueeze(1).to_broadcast([128, 4, 128]), op=ALU.mult),
                        r=[PK[bank], "cst"], w=[f"PTa{d}"])
            nslot = 0
            for c in range(NCH):
                for d in range(2):
                    bank = 2 + nslot % 4
                    nslot += 1
                    S.op("pe", lambda e, d=d, c=c, bank=bank: e.matmul(ps[bank][:, 0:129], lhsT=ktok[:, c, :], rhs=Vp[d][:, c, :],
                                                                       start=True, stop=True), r=["ktok", f"Vp{d}"], w=[PK[bank]])
                    S.op("act", lambda e, d=d, c=c, bank=bank: e.activation(out=Z[d][:, c, :], in_=ps[bank][:, 0:129],
                                                                            func=AF.Identity, scale=gsm[:, 8 + d, c:c + 1]),
                         r=[PK[bank], f"g_eF{d}"], w=[f"Z{d}"])
            for stp in range(1, NCH):
                cf = stp
                cb = NCH - 1 - stp
                S.op("dve", lambda e, cf=cf: e.scalar_tensor_tensor(out=Z[0][:, cf, :], in0=Z[0][:, cf - 1, :],
                                                                    scalar=gsm[:, 8, cf:cf + 1], in1=Z[0][:, cf, :],
                                                                    op0=ALU.mult, op1=ALU.add), r=["Z0", "g_eF0"], w=["Z0"])
                S.op("dve", lambda e, cb=cb: e.scalar_tensor_tensor(out=Z[1][:, cb, :], in0=Z[1][:, cb + 1, :],
                                                                    scalar=gsm[:, 9, cb:cb + 1], in1=Z[1][:, cb, :],
                                                                    op0=ALU.mult, op1=ALU.add), r=["Z1", "g_eF1"], w=["Z1"])
            S.op("dve", lambda e: e.tensor_copy(out=Zb[0][:, 1:NCH, :], in_=Z[0][:, 0:NCH - 1, :]), r=["Z0"], w=["Zb0"])
            S.op("act", lambda e: e.copy(out=Zb[1][:, 1:NCH, :], in_=Z[1][:, 1:NCH, :]), r=["Z1"], w=["Zb1"])
            slot = 0
            for c0 in range(0, NCH, 3):
                ncs = min(3, NCH - c0)
                for d in range(2):
                    bank = 2 + slot % 6
                    slot += 1
                    for cc in range(ncs):
                        c = c0 + cc
                        cs = slice(c * 128, (c + 1) * 128)
                        zi = c if d == 0 else c + 1
                        S.op("pe", lambda e, d=d, c=c, cc=cc, bank=bank: e.matmul(
                            ps[bank][:, cc * 129:(cc + 1) * 129], lhsT=PTa[d][:, c, :], rhs=Vp[d][:, c, :], start=True, stop=False),
                            r=[f"PTa{d}", f"Vp{d}"], w=[PK[bank]])
                        S.op("pe", lambda e, d=d, cs=cs, cc=cc, bank=bank, zi=zi: e.matmul(
                            ps[bank][:, cc * 129:(cc + 1) * 129], lhsT=qT[:, cs], rhs=Zb[d][:, zi, :], start=False, stop=True),
                            r=["qT", f"Zb{d}"], w=[PK[bank]])
                    src = ps[bank][:, 0:ncs * 129].rearrange("p (c n) -> p c n", c=ncs)
                    if slot % 2 == 0:
                        S.op("act", lambda e, d=d, c0=c0, ncs=ncs, src=src: e.copy(out=Z[d][:, c0:c0 + ncs, :], in_=src),
                             r=[PK[bank], f"Zb{d}"], w=[f"Z{d}"])
                    else:
                        S.op("dve", lambda e, d=d, c0=c0, ncs=ncs, src=src: e.tensor_copy(out=Z[d][:, c0:c0 + ncs, :], in_=src),
                             r=[PK[bank], f"Zb{d}"], w=[f"Z{d}"])
            hraw = Z
            for d in range(2):
                den = hraw[d][:, :, 128]
                S.op("dve", lambda e, d=d, den=den: e.tensor_tensor(out=gsm[:, 12 + d, :], in0=den, in1=gsm[:, 2 + d, :],
                                                                   op=ALU.mult), r=[f"Z{d}", f"g_rs{d}"], w=[f"g_cf{d}"])
                S.op("dve", lambda e, d=d: e.tensor_scalar(out=gsm[:, 15, :], in0=gsm[:, 12 + d, :], scalar1=-1.0,
                                                           scalar2=1.0, op0=ALU.mult, op1=ALU.max),
                     r=[f"g_cf{d}"], w=["g_neg"])
                S.op("dve", lambda e, d=d: e.tensor_tensor(out=gsm[:, 12 + d, :], in0=gsm[:, 12 + d, :], in1=gsm[:, 15, :],
                                                           op=ALU.max), r=[f"g_cf{d}", "g_neg"], w=[f"g_cf{d}"])
                S.op("dve", lambda e, d=d: e.reciprocal(out=gsm[:, 12 + d, :], in_=gsm[:, 12 + d, :]),
                     r=[f"g_cf{d}"], w=[f"g_cf{d}"])
                S.op("dve", lambda e, d=d: e.tensor_tensor(out=gsm[:, 12 + d, :], in0=gsm[:, 12 + d, :], in1=gsm[:, 2 + d, :],
                                                           op=ALU.mult), r=[f"g_cf{d}", f"g_rs{d}"], w=[f"g_cf{d}"])
            bc = lambda ap: ap.unsqueeze(2).to_broadcast([128, NCH, 128])
            z1v = Z[1][:, :, 0:128]
            S.op("dve", lambda e: e.tensor_tensor(out=hs[:], in0=Z[0][:, :, 0:128], in1=bc(gsm[:, 12, :]), op=ALU.mult),
                 r=["Z0", "g_cf0"], w=["hs"])
            S.op("pool", lambda e: e.tensor_tensor(out=z1v, in0=z1v, in1=bc(gsm[:, 13, :]), op=ALU.mult),
                 r=["Z1", "g_cf1"], w=["Z1"])
            S.op("dve", lambda e: e.tensor_tensor(out=hs[:], in0=hs[:], in1=z1v, op=ALU.add), r=["hs", "Z1"], w=["hs"])
            S.op("dve", lambda e: e.tensor_tensor(out=z1v, in0=hs[:], in1=hs[:], op=ALU.mult), r=["hs", "Z1"], w=["Z1"])
            S.op("dve", lambda e: e.tensor_reduce(out=gsm[:, 14, :], in_=z1v, axis=AX.X, op=ALU.add),
                 r=["Z1"], w=["g_ss"])
            S.op("act", lambda e: e.activation(out=gsm[:, 14, :], in_=gsm[:, 14, :], func=AF.Sqrt, scale=1.0 / 128, bias=1e-6),
                 r=["g_ss"], w=["g_ss"])
            S.op("dve", lambda e: e.reciprocal(out=gsm[:, 14, :], in_=gsm[:, 14, :]), r=["g_ss"], w=["g_ss"])
            S.op("dve", lambda e: e.tensor_tensor(out=hs[:], in0=hs[:], in1=bc(gsm[:, 14, :]), op=ALU.mult),
                 r=["hs", "g_ss"], w=["hs"])
            S.op("dve", lambda e, h=h: e.tensor_tensor(out=hs[:], in0=hs[:],
                                                        in1=ghead[:, h * 128:(h + 1) * 128].unsqueeze(1).to_broadcast([128, NCH, 128]),
                                                        op=ALU.mult), r=["hs", "ghead"], w=["hs"])
            S.op("dve", lambda e: e.tensor_tensor(out=ybf[:], in0=hs[:], in1=og_t[:], op=ALU.mult),
                 r=["hs", "og_t"], w=["ybf"])
            S.dma("pool", lambda e, b=b, h=h: e.dma_start(
                out=y_d[b * SEQ:(b + 1) * SEQ, h * 128:(h + 1) * 128].rearrange("(c p) d -> p c d", p=128), in_=ybf[:]),
                r=["ybf"], w=["y_d"], key="ybf")
    S.barrier()
    A.reset(PM)
    if stage == -2:
        return nc, S
    w_out_b = A("w_out", [128, 8, D], BF16)
    w_q_b = A("w_q", [128, 8, D], BF16)
    w_o_b = A("w_o", [128, 8, D], BF16)
    w_r_b = A("w_r", [128, 8, 36], BF16)
    vecB = A("vecB", [128, 3 * 1024 + 36 + 32], F32)
    gx = vecB[:, 0:1024]
    gmem = vecB[:, 1024:2048]
    gmoe = vecB[:, 2048:3072]
    b_r = vecB[:, 3072:3108]
    iota = vecB[:, 3108:3140]
    S.dma("sp", lambda e: e.dma_start(out=vecB[:, 0:3072], in_=vec_d[:, VOFF["g_xattn"][0]:VOFF["g_xattn"][0] + 3072]), w=["vecB"], key="vecB")
    S.dma("sp", lambda e: e.dma_start(out=vecB[:, 3072:3140], in_=vec_d[:, VOFF["b_r"][0]:VOFF["b_r"][0] + 68]), w=["vecB"], key="vecB")
    load_w(w_r_b, w_r_d, "w_r", 8)

    xtsB = [A("xtB", [128, D], F32) for _ in range(2)]
    yts = [A("ytB", [128, D], BF16) for _ in range(2)]
    yT = A("yT", [128, 8, 128], BF16)
    x1 = A("x1", [128, 4, D], F32)
    junkB = A("junkB", [128, D], BF16)
    ssB = A("ssB", [128, 1], F32)
    rstdB = A("rstdB", [128, 1], F32)
    xn1 = A("xn1", [128, D], BF16)
    xn1T = A("xn1T", [128, 8, 512], BF16)
    qTx = A("qTx", [128, 8, 512], BF16)
    hmT = A("hmT", [128, 8, 256], BF16)
    kmT = [A("kmT", [128, 8, 256], BF16) for _ in range(2)]
    vm = [A("vm", [128, 2, D], BF16) for _ in range(2)]
    pT = [A("pT", [128, 512], BF16) for _ in range(2)]
    rden = A("rden", [128, 512], F32)
    ocT = A("ocT", [128, 8, 512], BF16)
    x2t = [A("x2t", [128, D], F32) for _ in range(2)]
    hbs = [A("hb", [128, D], BF16) for _ in range(4)]
    yT_c = A("yT", [128, 8, 128], BF16)
    yT_d = A("yT", [128, 8, 128], BF16)
    ss4 = A("ss4", [128, 4], F32)
    rstd4 = A("rstd4", [128, 4], F32)
    run_bc = A("run_bc", [128, 32], F32)
    S.op("pool", lambda e: e.memset(run_bc[:], 0.0), w=["run_bc"])
    KVM = A.mark()
    w_kv_b = A("w_kv", [128, 8, 2 * D], BF16)
    stgB = [(x1[:, 0:2, :].rearrange("p a n -> p (a n)"), "stgB0"), (x1[:, 2:4, :].rearrange("p a n -> p (a n)"), "stgB1"),
            (ocT[:].rearrange("p a n -> p (a n)").bitcast(F32), "stgB2"), (qTx[:].rearrange("p a n -> p (a n)").bitcast(F32), "stgB3"),
            (xn1T[:].rearrange("p a n -> p (a n)").bitcast(F32), "stgB4")]
    load_w_fast(w_kv_b, w_kv_d, "w_kv", 8, stgB)

    for b in range(2):
        for mt in range(2):
            p = mt
            S.dma("sp", lambda e, b=b, mt=mt, p=p: e.dma_start(out=xtsB[p][:], in_=mem_d[b * 256 + mt * 128: b * 256 + (mt + 1) * 128, :]),
                  w=[f"xtB{p}"], key=f"xtB{p}")
            rms_to_bf16(xtsB[p][:], f"xtB{p}", gmem, "vecB", xn1[:], "xn1", junkB, ssB, rstdB, "B")
            transpose8(xn1, "xn1", hmT[:, :, mt * 128:(mt + 1) * 128], "hmT", 0)
        for fc in range(8):
            bank = 1 + fc % 2
            for kc in range(8):
                S.op("pe", lambda e, fc=fc, kc=kc, bank=bank: e.matmul(
                    ps[bank][:, 0:256], lhsT=w_kv_b[:, kc, fc * 128:(fc + 1) * 128], rhs=hmT[:, kc, :],
                    start=(kc == 0), stop=(kc == 7)), r=["w_kv", "hmT"], w=[PK[bank]])
            S.op("act", lambda e, fc=fc, b=b, bank=bank: e.copy(out=kmT[b][:, fc, :], in_=ps[bank][:, 0:256]),
                 r=[PK[bank]], w=[f"kmT{b}"])
        for mt in range(2):
            for half in range(2):
                bank = 3 + half
                for kc in range(8):
                    S.op("pe", lambda e, mt=mt, half=half, kc=kc, bank=bank: e.matmul(
                        ps[bank][:, :], lhsT=hmT[:, kc, mt * 128:(mt + 1) * 128],
                        rhs=w_kv_b[:, kc, 1024 + half * 512:1024 + (half + 1) * 512],
                        start=(kc == 0), stop=(kc == 7)), r=["w_kv", "hmT"], w=[PK[bank]])
                S.op("dve", lambda e, mt=mt, half=half, b=b, bank=bank: e.tensor_copy(
                    out=vm[b][:, mt, half * 512:(half + 1) * 512], in_=ps[bank][:, :]), r=[PK[bank]], w=[f"vm{b}"])

    load_w_fast(w_out_b, w_out_d, "w_out", 8, stgB)
    load_w_fast(w_q_b, w_q_d, "w_q", 8, stgB)
    load_w_fast(w_o_b, w_o_d, "w_o", 8, stgB)
    XSC = 256 ** -0.5
    S.barrier()
    A.reset(KVM)
    xtsB3 = xtsB + [A("xtB", [128, D], F32)]
    yts3 = yts + [A("ytB", [128, D], BF16)]
    yT2 = [yT, A("yT", [128, 8, 128], BF16)]
    yT4 = yT2 + [yT_c, yT_d]
    xn1s = [xn1, A("xn1", [128, D], BF16)]
    x2t4 = x2t + [A("x2t", [128, D], F32) for _ in range(2)]
    hbs8 = hbs + [A("hb", [128, D], BF16) for _ in range(4)]
    KVM_R = A.mark()

    def load_xy_x(i):
        p = i % 3
        S.dma("sp", lambda e: e.dma_start(out=xtsB3[p][:], in_=x_d[i * 128:(i + 1) * 128, :]), w=[f"xtB{p}"], key=f"xtB{p}")

    def load_xy_y(i):
        p = i % 3
        S.dma("sp", lambda e: e.dma_start(out=yts3[p][:], in_=y_d[i * 128:(i + 1) * 128, :]), w=[f"ytB{p}"], key=f"ytB{p}")

    def load_xy(i):
        load_xy_x(i)
        load_xy_y(i)

    hT2 = yT2

    A.reset(KVM_R)
    rsegs = [A("rseg", [128, 4, 288], F32) for _ in range(2)]
    rAbs = [A("rAbs", [128, 4, 32], BF16) for _ in range(2)]

    def router_chain(seg):
        q2 = seg % 2
        rs = rsegs[q2]
        R = [f"rseg{q2}"]
        V = lambda a, b_: rs[:, :, a:b_]
        S.op("dve", lambda e: e.tensor_tensor(out=V(0, 36), in0=ps[5][:, 256:400].rearrange("p (t n) -> p t n", t=4),
                                              in1=b_r.unsqueeze(1).to_broadcast([128, 4, 36]), op=ALU.add),
             r=["ps5r0", "ps5r1", "ps5r2", "ps5r3", "vecB"], w=R)
        S.op("dve", lambda e: e.tensor_reduce(out=rs[:, :, 40], in_=V(0, 4), axis=AX.X, op=ALU.max), r=R, w=R)
        S.op("dve", lambda e: e.tensor_tensor(out=V(52, 56), in0=V(0, 4), in1=V(40, 41).to_broadcast([128, 4, 4]), op=ALU.subtract), r=R, w=R)
        S.op("act", lambda e: e.activation(out=V(52, 56), in_=V(52, 56), func=AF.Exp), r=R, w=R)
        S.op("dve", lambda e: e.tensor_reduce(out=rs[:, :, 42], in_=V(52, 56), axis=AX.X, op=ALU.add), r=R, w=R)
        S.op("dve", lambda e: e.reciprocal(out=V(43, 44), in_=V(42, 43)), r=R, w=R)
        S.op("dve", lambda e: e.tensor_tensor(out=V(44, 48), in0=V(0, 4), in1=V(40, 41).to_broadcast([128, 4, 4]), op=ALU.is_equal), r=R, w=R)
        S.op("dve", lambda e: e.tensor_scalar(out=V(48, 52), in0=V(44, 48), scalar1=1e9, scalar2=-1e9, op0=ALU.mult, op1=ALU.add), r=R, w=R)
        S.op("dve", lambda e: e.tensor_tensor(out=V(56, 88).rearrange("p t (g k) -> p t g k", g=4),
                                              in0=V(4, 36).rearrange("p t (g k) -> p t g k", g=4),
                                              in1=V(48, 52).unsqueeze(3).to_broadcast([128, 4, 4, 8]), op=ALU.add), r=R, w=R)
        for t in range(4):
            S.op("dve", lambda e, t=t: e.max(out=rs[:, t, 88:96], in_=rs[:, t, 56:88]), r=R, w=R)
            S.op("dve", lambda e, t=t: e.max_index(out=rs[:, t, 96:104].bitcast(U32), in_max=rs[:, t, 88:96], in_values=rs[:, t, 56:88]), r=R, w=R)
        for t in range(4):
            S.op("dve", lambda e, t=t: e.tensor_copy(out=rs[:, t, 104:106], in_=rs[:, t, 96:98].bitcast(U32)), r=R, w=R)
        S.op("dve", lambda e: e.tensor_tensor(out=V(106, 107), in0=V(88, 89), in1=V(89, 90), op=ALU.subtract), r=R, w=R)
        S.op("act", lambda e: e.activation(out=V(107, 108), in_=V(106, 107), func=AF.Sigmoid), r=R, w=R)
        S.op("dve", lambda e: e.tensor_tensor(out=V(108, 109), in0=V(107, 108), in1=V(43, 44), op=ALU.mult), r=R, w=R)
        S.op("dve", lambda e: e.tensor_tensor(out=V(109, 110), in0=V(43, 44), in1=V(108, 109), op=ALU.subtract), r=R, w=R)
        for k in range(2):
            S.op("dve", lambda e, k=k: e.tensor_tensor(out=V(112 + 32 * k, 144 + 32 * k), in0=iota.unsqueeze(1).to_broadcast([128, 4, 32]),
                                                       in1=V(104 + k, 105 + k).to_broadcast([128, 4, 32]), op=ALU.is_equal),
                 r=R + ["vecB"], w=R)
        S.op("dve", lambda e: e.tensor_tensor(out=rAbs[q2][:], in0=V(112, 144), in1=V(144, 176), op=ALU.add), r=R, w=R)

    def router_tail_seg(seg):
        q2 = seg % 2
        rs = rsegs[q2]
        R = [f"rseg{q2}"]
        V = lambda a, b_: rs[:, :, a:b_]
        Ab = rAbs[q2]
        for t in range(4):
            S.op("pe", lambda e, t=t: e.matmul(ps[5][:, 32 * t:32 * t + 32], lhsT=strict_b[:], rhs=Ab[:, t, :], start=True, stop=(t == 0)),
                 r=["strictb"] + R, w=["ps5k"])
            for t2 in range(t):
                S.op("pe", lambda e, t=t, t2=t2: e.matmul(ps[5][:, 32 * t:32 * t + 32], lhsT=ones_b[:], rhs=Ab[:, t2, :],
                                                          start=False, stop=(t2 == t - 1)), r=["onesb"] + R, w=["ps5k"])
        for t in range(4):
            S.op("pe", lambda e, t=t: e.matmul(ps[5][:, 128:160], lhsT=ones_b[:], rhs=Ab[:, t, :], start=(t == 0), stop=(t == 3)),
                 r=["onesb"] + R, w=["ps5k"])
        S.op("dve", lambda e: e.tensor_tensor(out=V(208, 240), in0=ps[5][:, 0:128].rearrange("p (t n) -> p t n", t=4),
                                              in1=run_bc[:].unsqueeze(1).to_broadcast([128, 4, 32]), op=ALU.add),
             r=["ps5k", "run_bc"], w=R)
        S.op("dve", lambda e: e.tensor_tensor(out=run_bc[:], in0=ps[5][:, 128:160], in1=run_bc[:], op=ALU.add),
             r=["ps5k", "run_bc"], w=["run_bc"])
        t0 = seg * 4
        for k in range(2):
            S.op("dve", lambda e, k=k: e.tensor_tensor(out=V(240, 272), in0=V(112 + 32 * k, 144 + 32 * k), in1=V(208, 240), op=ALU.mult), r=R, w=R)
            S.op("dve", lambda e, k=k: e.tensor_reduce(out=rs[:, :, 272 + k], in_=V(240, 272), axis=AX.X, op=ALU.add), r=R, w=R)
            S.op("dve", lambda e, k=k: e.scalar_tensor_tensor(out=V(274 + k, 275 + k), in0=V(104 + k, 105 + k), scalar=float(CAP),
                                                              in1=V(272 + k, 273 + k), op0=ALU.mult, op1=ALU.add), r=R, w=R)
            S.op("dve", lambda e, k=k: e.tensor_single_scalar(out=V(276 + k, 277 + k), in_=V(272 + k, 273 + k), scalar=float(CAP),
                                                              op=ALU.is_ge), r=R, w=R)
            S.op("dve", lambda e, k=k: e.scalar_tensor_tensor(out=V(274 + k, 275 + k), in0=V(276 + k, 277 + k), scalar=1.0e6,
                                                              in1=V(274 + k, 275 + k), op0=ALU.mult, op1=ALU.add), r=R, w=R)
            S.op("dve", lambda e, k=k: e.tensor_tensor(out=V(278 + k, 279 + k), in0=V(108 + k, 109 + k), in1=V(276 + k, 277 + k),
                                                       op=ALU.mult), r=R, w=R)
            S.op("dve", lambda e, k=k: e.tensor_tensor(out=wts[:, t0:t0 + 4, k:k + 1], in0=V(108 + k, 109 + k), in1=V(278 + k, 279 + k),
                                                       op=ALU.subtract), r=R, w=["wts"])
        S.op("dve", lambda e: e.tensor_copy(out=slots_i[:, t0:t0 + 4, :], in_=V(274, 276)), r=R, w=["slots"])
        for t in range(4):
            i = t0 + t
            hq = i % 8
            for k in range(2):
                S.dma("pool", lambda e, k=k, i=i, hq=hq: e.indirect_dma_start(
                    out=xd_d[:, :], out_offset=bass.IndirectOffsetOnAxis(ap=slots_i[:, i, k:k + 1], axis=0),
                    in_=hbs8[hq][:, :], in_offset=None, bounds_check=_bcreg(e), oob_is_err=False),
                    r=[f"hb{hq}", "slots"], w=["xd", "scat"], key=f"sc{hq}")

    TB = (0, 6)

    def rms4_stats(srcs):
        for j_, (ap_, k_) in enumerate(srcs):
            S.op("act", lambda e, ap_=ap_, j_=j_: e.activation(out=junkB[:], in_=ap_, func=AF.Square, accum_out=ss4[:, j_:j_ + 1]),
                 r=[k_], w=["junkB_", f"ss4_{j_}"])
        S.op("act", lambda e: e.activation(out=rstd4[:], in_=ss4[:], func=AF.Sqrt, scale=1.0 / D, bias=1e-6),
             r=[f"ss4_{j_}" for j_ in range(4)], w=["rstd4"])
        S.op("dve", lambda e: e.reciprocal(out=rstd4[:], in_=rstd4[:]), r=["rstd4"], w=["rstd4"])

    def rms4_apply(j_, src, g_ap, dst):
        (ap_, k_), (dap_, dk_) = src, dst
        S.op("dve", lambda e: e.scalar_tensor_tensor(out=dap_, in0=ap_, scalar=rstd4[:, j_:j_ + 1], in1=g_ap,
                                                     op0=ALU.mult, op1=ALU.mult), r=[k_, "rstd4", "vecB"], w=[dk_])

    pend_chain = []
    pend_tail = []
    load_xy(0)
    load_xy(1)
    load_xy(2)
    for seg in range(NT // 4):
        b = seg // 8
        for j in range(4):
            i = seg * 4 + j
            p = i % 3
            transpose8(yts3[p], f"ytB{p}", yT4[j][:], f"yT{j}", TB[j % 2], evac="act")
            if i + 3 < NT:
                load_xy_y(i + 3)
        for j in range(4):
            i = seg * 4 + j
            p = i % 3
            for half in range(2):
                bank = (1 if j % 2 == 0 else 3) + half
                for kc in range(8):
                    S.op("pe", lambda e, half=half, kc=kc, bank=bank, j=j: e.matmul(
                        ps[bank][:, :], lhsT=yT4[j][:, kc, :], rhs=w_out_b[:, kc, half * 512:(half + 1) * 512],
                        start=(kc == 0), stop=(kc == 7)), r=[f"yT{j}", "w_out"], w=[PK[bank]])
                S.op("dve", lambda e, half=half, j=j, p=p, bank=bank: e.tensor_tensor(
                    out=x1[:, j, half * 512:(half + 1) * 512], in0=ps[bank][:, :], in1=xtsB3[p][:, half * 512:(half + 1) * 512],
                    op=ALU.add), r=[PK[bank], f"xtB{p}"], w=[f"x1_{j}"])
            if i + 3 < NT:
                load_xy_x(i + 3)
            if stage == 1:
                S.dma("pool", lambda e, i=i, j=j: e.dma_start(out=out_d[i * 128:(i + 1) * 128, :], in_=x1[:, j, :]),
                      r=[f"x1_{j}"], w=[f"out{i}"], key=f"x1o{j}")
            if j == 0:
                while pend_chain:
                    pend_chain.pop(0)()
        if stage == 1:
            continue
        rms4_stats([(x1[:, j, :], f"x1_{j}") for j in range(4)])
        for jp in range(2):
            for j in (2 * jp, 2 * jp + 1):
                rms4_apply(j, (x1[:, j, :], f"x1_{j}"), gx, (xn1s[j % 2][:], f"xn1_{j % 2}"))
            for j in (2 * jp, 2 * jp + 1):
                transpose8(xn1s[j % 2], f"xn1_{j % 2}", xn1T[:, :, j * 128:(j + 1) * 128], f"xn1T_{j}", TB[j % 2],
                           evac=("act" if j % 2 == 0 else "dve"))
        while pend_tail:
            pend_tail.pop(0)()
        for fc in range(8):
            bank = 1 + fc % 2
            for kc in range(8):
                S.op("pe", lambda e, fc=fc, kc=kc, bank=bank: e.matmul(
                    ps[bank][:, :], lhsT=w_q_b[:, kc, fc * 128:(fc + 1) * 128], rhs=xn1T[:, kc, :],
                    start=(kc == 0), stop=(kc == 7)), r=[f"xn1T_{jx}" for jx in range(4)] + ["w_q"], w=[PK[bank]])
            S.op("act", lambda e, fc=fc, bank=bank: e.copy(out=qTx[:, fc, :], in_=ps[bank][:, :]), r=[PK[bank]], w=["qTx"])
        for h in range(4):
            for mt in range(2):
                bank = 3 + mt
                for dc in range(2):
                    S.op("pe", lambda e, h=h, mt=mt, dc=dc, bank=bank, b=b: e.matmul(
                        ps[bank][:, :], lhsT=kmT[b][:, h * 2 + dc, mt * 128:(mt + 1) * 128], rhs=qTx[:, h * 2 + dc, :],
                        start=(dc == 0), stop=(dc == 1)), r=[f"kmT{b}", "qTx"], w=[PK[bank]])
                S.op("act", lambda e, mt=mt, bank=bank: e.activation(out=pT[mt][:], in_=ps[bank][:, :], func=AF.Exp, scale=XSC),
                     r=[PK[bank]], w=[f"pT{mt}"])
            for mt in range(2):
                S.op("pe", lambda e, mt=mt: e.matmul(ps[5][:, :], lhsT=ones_b[:], rhs=pT[mt][:], start=(mt == 0), stop=(mt == 1)),
                     r=["onesb", f"pT{mt}"], w=["ps5r0", "ps5r1", "ps5r2", "ps5r3", "ps5k"])
            S.op("dve", lambda e: e.reciprocal(out=rden[:], in_=ps[5][:, :]), r=["ps5r0", "ps5k"], w=["rden"])
            for ec in range(2):
                bank = 6 + ec
                for mt in range(2):
                    S.op("pe", lambda e, h=h, ec=ec, mt=mt, bank=bank, b=b: e.matmul(
                        ps[bank][:, :], lhsT=vm[b][:, mt, h * 256 + ec * 128:h * 256 + (ec + 1) * 128], rhs=pT[mt][:],
                        start=(mt == 0), stop=(mt == 1)), r=[f"vm{b}", f"pT{mt}"], w=[PK[bank]])
                S.op("dve", lambda e, h=h, ec=ec, bank=bank: e.tensor_tensor(out=ocT[:, h * 2 + ec, :], in0=ps[bank][:, :],
                                                                             in1=rden[:], op=ALU.mult),
                     r=[PK[bank], "rden"], w=["ocT"])
        for j in range(4):
            i = seg * 4 + j
            p4 = i % 4
            for half in range(2):
                bank = 1 + half
                for kc in range(8):
                    S.op("pe", lambda e, half=half, kc=kc, bank=bank, j=j: e.matmul(
                        ps[bank][:, :], lhsT=ocT[:, kc, j * 128:(j + 1) * 128], rhs=w_o_b[:, kc, half * 512:(half + 1) * 512],
                        start=(kc == 0), stop=(kc == 7)), r=["ocT", "w_o"], w=[PK[bank]])
                S.op("dve", lambda e, half=half, j=j, p4=p4, bank=bank: e.tensor_tensor(
                    out=x2t4[p4][:, half * 512:(half + 1) * 512], in0=ps[bank][:, :], in1=x1[:, j, half * 512:(half + 1) * 512],
                    op=ALU.add), r=[PK[bank], f"x1_{j}"], w=[f"x2t{p4}"])
            S.dma("sp", lambda e, i=i, p4=p4: e.dma_start(out=out_d[i * 128:(i + 1) * 128, :], in_=x2t4[p4][:]),
                  r=[f"x2t{p4}"], w=[f"out{i}"], key=f"x2t{p4}")
        if stage == 2:
            continue
        rms4_stats([(x2t4[(seg * 4 + j) % 4][:], f"x2t{(seg * 4 + j) % 4}") for j in range(4)])
        for j in range(4):
            rms4_apply(j, (x2t4[(seg * 4 + j) % 4][:], f"x2t{(seg * 4 + j) % 4}"), gmoe,
                       (hbs8[(seg * 4 + j) % 8][:], f"hb{(seg * 4 + j) % 8}"))
        for j in range(4):
            i = seg * 4 + j
            hq = i % 8
            transpose8(hbs8[hq], f"hb{hq}", yT4[j][:], f"yT{j}", TB[j % 2], evac=("act" if j % 2 == 0 else "dve"))
            for kc in range(8):
                S.op("pe", lambda e, kc=kc, j=j: e.matmul(ps[5][:, 256 + 36 * j:292 + 36 * j], lhsT=yT4[j][:, kc, :], rhs=w_r_b[:, kc, :],
                                                          start=(kc == 0), stop=(kc == 7)), r=[f"yT{j}", "w_r"], w=[f"ps5r{j}"])
        pend_chain.append(lambda seg=seg: router_chain(seg))
        pend_tail.append(lambda seg=seg: router_tail_seg(seg))
    while pend_chain:
        pend_chain.pop(0)()
    while pend_tail:
        pend_tail.pop(0)()
    if stage >= 2.5:
        S.op("dve", lambda e: e.tensor_copy(out=cnt_i[:], in_=run_bc[:]), r=["run_bc"], w=["cnt_i"])
    if debug and stage >= 2.5:
        ds_d = nc.dram_tensor("dbg_slots", [128, NT * 2], I32, kind="ExternalOutput").ap()
        dw_d = nc.dram_tensor("dbg_wts", [128, NT * 2], F32, kind="ExternalOutput").ap()
        S.dma("sp", lambda e: e.dma_start(out=ds_d, in_=slots_i[:].rearrange("p t k -> p (t k)")), r=["slots"], w=["dbgs"], key="dbgs")
        S.dma("sp", lambda e: e.dma_start(out=dw_d, in_=wts[:].rearrange("p t k -> p (t k)")), r=["wts"], w=["dbgw"], key="dbgw")
    S.barrier()
    A.reset(PM)
    if stage < 2.7:
        return nc, S

    wg = [A("wg", [128, 8, 512], BF16) for _ in range(2)]
    wu = [A("wu", [128, 8, 512], BF16) for _ in range(2)]
    wd = [A("wd", [128, 4, D], BF16) for _ in range(2)]
    xbs = [A("xb", [128, D], BF16) for _ in range(6)]
    xbT = [A("xbT", [128, 8, 512], BF16) for _ in range(2)]
    sgt = [A("sgt", [128, 512], F32) for _ in range(2)]
    hmid = [A("hmid", [128, 4, 512], BF16) for _ in range(2)]
    yst = [A("yst", [128, D], F32) for _ in range(3)]

    stg_g = [A("stg_g", [128, 8, 512], F32) for _ in range(2)]
    stg_u = [A("stg_u", [128, 8, 512], F32) for _ in range(2)]
    stg_d = [A("stg_d", [128, 4, D], F32) for _ in range(2)]

    def load_expert_dma(e_):
        q = e_ % 2
        S.dma("act", lambda e: e.dma_start(out=stg_g[q][:], in_=w_gate_d[e_].rearrange("(c p) n -> p c n", p=128)),
              w=[f"stg_g{q}"], key=f"stg_g{q}")
        S.dma("act", lambda e: e.dma_start(out=stg_u[q][:], in_=w_up_d[e_].rearrange("(c p) n -> p c n", p=128)),
              w=[f"stg_u{q}"], key=f"stg_u{q}")
        S.dma("act", lambda e: e.dma_start(out=stg_d[q][:], in_=w_down_d[e_].rearrange("(c p) n -> p c n", p=128)),
              w=[f"stg_d{q}"], key=f"stg_d{q}")

    def load_expert_cast(e_):
        q = e_ % 2
        for hh in range(2):
            S.op("dve", lambda e, hh=hh: e.tensor_copy(out=wg[q][:, hh * 4:(hh + 1) * 4, :], in_=stg_g[q][:, hh * 4:(hh + 1) * 4, :]),
                 r=[f"stg_g{q}"], w=[f"wg{q}"])
            S.op("act", lambda e, hh=hh: e.copy(out=wu[q][:, hh * 4:(hh + 1) * 4, :], in_=stg_u[q][:, hh * 4:(hh + 1) * 4, :]),
                 r=[f"stg_u{q}"], w=[f"wu{q}"])
            S.op("pool", lambda e, hh=hh: e.tensor_copy(out=wd[q][:, hh * 2:(hh + 1) * 2, :], in_=stg_d[q][:, hh * 2:(hh + 1) * 2, :]),
                 r=[f"stg_d{q}"], w=[f"wd{q}"])

    groups = []
    off = 0
    while off < CAP:
        n = min(512 if off == 0 else 256, CAP - off)
        groups.append((off, n))
        off += n
    allg = [(ex_, goff_, n_) for ex_ in range(32) for (goff_, n_) in groups]
    SEC_START = tuple(g_[0] for g_ in groups[1:])
    SEC_END = SEC_START
    NXB = 6
    xb_of = {}
    xbi = 0

    def issue_loads(k):
        nonlocal xbi
        ex_, goff_, n_ = allg[k]
        ids = []
        for blk_ in range(n_ // 128):
            xq_ = xbi % NXB
            xbi += 1
            r0_ = ex_ * CAP + goff_ + blk_ * 128
            S.dma("sp", lambda e, xq_=xq_, r0_=r0_: e.dma_start(out=xbs[xq_][:], in_=xd_d[r0_:r0_ + 128, :]),
                  w=[f"xb{xq_}"], key=f"xb{xq_}")
            ids.append(xq_)
        xb_of[k] = ids

    load_expert_dma(0)
    load_expert_dma(1)
    load_expert_cast(0)
    issue_loads(0)
    gi_ = 0
    ysi = 0
    for k, (ex, goff, n) in enumerate(allg):
        q = ex % 2
        if k + 1 < len(allg):
            issue_loads(k + 1)
        if goff == groups[1][0]:
            if ex + 1 < 32:
                load_expert_cast(ex + 1)
            if ex + 2 < 32:
                load_expert_dma(ex + 2)
        if goff in SEC_START and DYN_SKIP:
            S.section_begin(cnt_i[0:1, ex:ex + 1], goff, tag=ex)
        if True:
            gq = gi_ % 2
            gi_ += 1
            nb = n // 128
            for blk in range(nb):
                xq = xb_of[k][blk]
                transpose8(xbs[xq], f"xb{xq}", xbT[gq][:, :, blk * 128:(blk + 1) * 128], f"xbT{gq}", blk % 2,
                           evac=("act" if blk % 2 == 0 else "dve"))
            for fc in range(4):
                for (wt, wkey, bank) in ((wg[q], f"wg{q}", 2 + (fc % 2)), (wu[q], f"wu{q}", 4 + (fc % 2))):
                    for kc in range(8):
                        S.op("pe", lambda e, wt=wt, fc=fc, kc=kc, bank=bank, gq=gq, n=n: e.matmul(
                            ps[bank][:, 0:n], lhsT=wt[:, kc, fc * 128:(fc + 1) * 128], rhs=xbT[gq][:, kc, 0:n],
                            start=(kc == 0), stop=(kc == 7)), r=[wkey, f"xbT{gq}"], w=[PK[bank]])
                sq = fc % 2
                S.op("act", lambda e, fc=fc, sq=sq, n=n: e.activation(out=sgt[sq][:, 0:n], in_=ps[2 + (fc % 2)][:, 0:n], func=AF.Silu),
                     r=[PK[2 + (fc % 2)]], w=[f"sgt{sq}"])
                S.op("dve", lambda e, fc=fc, sq=sq, n=n, gq=gq: e.tensor_tensor(out=hmid[gq][:, fc, 0:n], in0=ps[4 + (fc % 2)][:, 0:n],
                                                                               in1=sgt[sq][:, 0:n], op=ALU.mult),
                     r=[PK[4 + (fc % 2)], f"sgt{sq}"], w=[f"hmid{gq}"])
            for blk in range(nb):
                yq = ysi % 3
                ysi += 1
                for half in range(2):
                    bank = 6 + half
                    for fc in range(4):
                        S.op("pe", lambda e, fc=fc, half=half, bank=bank, gq=gq, blk=blk, q=q: e.matmul(
                            ps[bank][:, :], lhsT=hmid[gq][:, fc, blk * 128:(blk + 1) * 128], rhs=wd[q][:, fc, half * 512:(half + 1) * 512],
                            start=(fc == 0), stop=(fc == 3)), r=[f"hmid{gq}", f"wd{q}"], w=[PK[bank]])
                    if half == 0:
                        S.op("act", lambda e, yq=yq, bank=bank: e.copy(out=yst[yq][:, 0:512], in_=ps[bank][:, :]),
                             r=[PK[bank]], w=[f"yst{yq}a"])
                    else:
                        S.op("dve", lambda e, yq=yq, bank=bank: e.tensor_copy(out=yst[yq][:, 512:1024], in_=ps[bank][:, :]),
                             r=[PK[bank]], w=[f"yst{yq}b"])
                r0 = ex * CAP + goff + blk * 128
                S.dma("sp", lambda e, yq=yq, r0=r0: e.dma_start(out=yd_d[r0:r0 + 128, :], in_=yst[yq][:]),
                      r=[f"yst{yq}a", f"yst{yq}b"], w=["yd"], key=f"yst{yq}")
        if goff in SEC_END and DYN_SKIP:
            S.section_end()
    S.barrier()
    A.reset(PM)
    if stage < 2.9:
        return nc, S

    gfin = A("gfin", [128, D], F32)
    S.dma("sp", lambda e: e.dma_start(out=gfin[:], in_=vec_d[:, VOFF["g_final"][0]:VOFF["g_final"][0] + 1024]), w=["gfin"], key="gfin")
    x2s = [A("x2s", [128, D], F32) for _ in range(3)]
    yas = [A("ya", [128, D], F32) for _ in range(3)]
    ybs = [A("yb", [128, D], F32) for _ in range(3)]
    ots = [A("ot", [128, D], F32) for _ in range(3)]
    junkD = A("junkD", [128, D], BF16)
    ssD = [A("ssD", [128, 1], F32) for _ in range(2)]
    for q in range(3):
        S.op("pool", lambda e, q=q: e.memset(yas[q][:], 0.0), w=[f"ya{q}"])
        S.op("pool", lambda e, q=q: e.memset(ybs[q][:], 0.0), w=[f"yb{q}"])

    def load_d(i):
        q = i % 3
        S.dma("sp", lambda e: e.dma_start(out=x2s[q][:], in_=out_d[i * 128:(i + 1) * 128, :]), r=[f"out{i}"], w=[f"x2s{q}"], key=f"x2s{q}")
        for k, (buf, nm) in enumerate(((yas, "ya"), (ybs, "yb"))):
            if stage == 2.9 or (stage == 2.95 and i >= 8):
                continue
            S.dma("pool", lambda e, buf=buf, k=k: e.indirect_dma_start(
                out=buf[q][:, :], out_offset=None, in_=yd_d[:, :],
                in_offset=bass.IndirectOffsetOnAxis(ap=slots_i[:, i, k:k + 1], axis=0),
                bounds_check=_bcreg(e), oob_is_err=False), r=["yd", "slots"], w=[f"{nm}{q}", f"gath{(2 * i + k) % 3}"], key=f"{nm}{q}")

    load_d(0)
    load_d(1)
    for i in range(NT):
        q = i % 3
        if i + 2 < NT:
            load_d(i + 2)
        S.op("dve", lambda e, q=q, i=i: e.scalar_tensor_tensor(out=x2s[q][:], in0=yas[q][:], scalar=wts[:, i, 0:1], in1=x2s[q][:],
                                                               op0=ALU.mult, op1=ALU.add), r=[f"ya{q}", f"x2s{q}", "wts"], w=[f"x2s{q}"])
        S.op("dve", lambda e, q=q, i=i: e.scalar_tensor_tensor(out=x2s[q][:], in0=ybs[q][:], scalar=wts[:, i, 1:2], in1=x2s[q][:],
                                                                op0=ALU.mult, op1=ALU.add), r=[f"yb{q}", f"x2s{q}", "wts"], w=[f"x2s{q}"])
        rms_to_bf16(x2s[q][:], f"x2s{q}", gfin[:], "gfin", ots[q][:], f"ot{q}", junkD, ssD[0], ssD[1], "D")
        S.dma("sp", lambda e, q=q, i=i: e.dma_start(out=out_d[i * 128:(i + 1) * 128, :], in_=ots[q][:]),
              r=[f"ot{q}"], w=[f"out{i}"], key=f"ot{q}")
    S.barrier()
    return nc, S


_CACHE = {}


def _get_program(stage=3):
    if stage not in _CACHE:
        nc, S = build(stage)
        st = ExitStack()
        S.emit(st)
        _CACHE[stage] = (nc, st)
    return _CACHE[stage][0]


def _host_tables(inputs):
    f = lambda k: np.asarray(inputs[k], dtype=np.float32)
    rows = {
        "g_mix": f("g_mix")[0], "g_xattn": f("g_xattn")[0], "g_mem": f("g_mem")[0], "g_moe": f("g_moe")[0],
        "g_final": f("g_final"), "ln_g": f("ln_v_g")[0], "ln_b": f("ln_v_b")[0], "g_head": f("g_head")[0],
        "gate_b": f("gate_b")[0], "b_r": np.concatenate([f("b_rg")[0], f("b_re")[0]]),
        "iota": np.arange(32, dtype=np.float32),
    }
    vec = np.zeros((128, NVEC), np.float32)
    for k, (o, l) in VOFF.items():
        vec[:, o:o + l] = rows[k][None, :]
    col = np.zeros((128, NCOL), np.float32)
    cw = f("conv_w")[0][:, 0, :]
    col[:, 0:40] = cw.reshape(5, 8, 128).transpose(2, 1, 0).reshape(128, 40)
    col[:, 40:48] = f("conv_b")[0].reshape(8, 128).T
    col[:, 48:52] = f("b_s")[0].T
    r = np.arange(128)
    cst = np.zeros((128, 640), np.float32)
    cst[:, 0:128] = np.eye(128)
    cst[:, 128:256] = (r[:, None] <= r[None, :])
    cst[:, 256:384] = (r[:, None] >= r[None, :])
    cst[:, 384:512] = 1.0
    cst[:, 512:640] = (r[:, None] < r[None, :])
    w_sT = np.ascontiguousarray(f("w_s")[0].transpose(2, 0, 1))
    w_r = np.ascontiguousarray(np.concatenate([f("w_rg")[0], f("w_re")[0]], axis=1))
    return vec, col, cst, w_sT, w_r


def make_in_maps(inputs):
    vec, col, cst, w_sT, w_r = _host_tables(inputs)
    f = lambda k: np.ascontiguousarray(np.asarray(inputs[k], dtype=np.float32)[0])
    shared = {
        "w_in": f("w_in"), "w_out": f("w_out"), "w_q": f("w_q_x"), "w_kv": f("w_kv_x"), "w_o": f("w_o_x"),
        "w_r": w_r, "w_gate": f("w_gate"), "w_up": f("w_up"), "w_down": f("w_down"),
        "w_sT": w_sT, "vec": vec, "col": col, "cst": cst,
    }
    x = np.asarray(inputs["x"], dtype=np.float32)
    mem = np.asarray(inputs["mem"], dtype=np.float32)
    maps = []
    for c in range(NCORES):
        m = dict(shared)
        m["x"] = np.ascontiguousarray(x[2 * c:2 * c + 2].reshape(NTOK, D))
        m["mem"] = np.ascontiguousarray(mem[2 * c:2 * c + 2].reshape(512, D))
        maps.append(m)
    return maps


def kernel(**inputs):
    nc = _get_program(3)
    maps = make_in_maps(inputs)
    res = run_bass_kernel_spmd(nc, maps, core_ids=list(range(NCORES)))
    out = np.concatenate([np.asarray(r["out"]).reshape(2, SEQ, D) for r in res.results], axis=0)
    return out.astype(np.float32)
```
